# Optimizing a Trainium2 kernel written in Bass

```python
import jax, jax.numpy as jnp
from jax import lax
import numpy as np

D_MODEL = 1024
BATCH = 8
SEQ = 4096
DEPTH = 1

RWKV_HEAD_DIM = 64
RWKV_DIM = D_MODEL // 2
RWKV_HEADS = RWKV_DIM // RWKV_HEAD_DIM
DECAY_RANK = 64
ICLR_RANK = 64
GATE_RANK = 128
GN_EPS = 64e-5
ATTN_HEAD_DIM = 64
ATTN_DIM = D_MODEL // 2
ATTN_HEADS = ATTN_DIM // ATTN_HEAD_DIM
MOBA_BLOCK = 256
MOBA_TOPK = 3
Q_BLOCK = 128
ROPE_THETA = 500000.0
ROPE_DIM = ATTN_HEAD_DIM // 4
NEG_INF = -1e30
N_BRANCHES = 2
N_EXPERTS = 32
TOP_K = 4
D_EXPERT = D_MODEL
SWIGLU_LIMIT = 7.0
SWIGLU_ALPHA = 1.702
EXPERT_ROW_BLOCK = 128
DEEPNORM_ALPHA = (2.0 * DEPTH) ** 0.25
DEEPNORM_BETA = (8.0 * DEPTH) ** -0.25
LN_EPS = 1e-5
RWKV_COLS = 3 * RWKV_DIM + DECAY_RANK + ICLR_RANK + GATE_RANK
ATTN_COLS = 3 * ATTN_DIM
GATE_COLS = N_BRANCHES * D_MODEL
IN_COLS = RWKV_COLS + ATTN_COLS + GATE_COLS
RWKV_SPLITS = (RWKV_DIM, 2 * RWKV_DIM, 3 * RWKV_DIM, 3 * RWKV_DIM + DECAY_RANK, 3 * RWKV_DIM + DECAY_RANK + ICLR_RANK)

kernel_name = 'rwkv7_moba_gated_hybrid_moe_block'


def layer_norm(x, g, b):
    xf = x.astype(jnp.float32)
    mu = jnp.mean(xf, axis=-1, keepdims=True)
    var = jnp.mean(jnp.square(xf - mu), axis=-1, keepdims=True)
    return ((xf - mu) * lax.rsqrt(var + LN_EPS) * g + b).astype(x.dtype)


def rwkv7_time_mix(p, shift_mix, decay_w0, decay_up, iclr_a0, iclr_up, gate_up, k_k, k_a, r_k, gn_g, gn_b):
    B, T, _ = p.shape
    H, N = RWKV_HEADS, RWKV_HEAD_DIM
    prev = jnp.pad(p[:, :-1], ((0, 0), (1, 0), (0, 0)))
    p = p + (prev - p) * shift_mix
    r, k, v, xw, xa, xg = jnp.split(p, RWKV_SPLITS, axis=-1)
    w_raw = (decay_w0 + jnp.tanh(xw) @ decay_up).astype(jnp.float32)
    decay = jnp.exp(-jnp.exp(-jax.nn.softplus(-w_raw) - 0.5))
    a = jax.nn.sigmoid(iclr_a0 + xa @ iclr_up)
    g = jax.nn.sigmoid(xg) @ gate_up
    kk = (k * k_k).reshape(B, T, H, N).astype(jnp.float32)
    kk = kk / jnp.maximum(jnp.linalg.norm(kk, axis=-1, keepdims=True), 1e-12)
    k = k * (1.0 + (a - 1.0) * k_a)

    def heads(t):
        return t.reshape(B, T, H, N).astype(jnp.float32)

    r_h, k_h, v_h, w_h, a_h = heads(r), heads(k), heads(v), heads(decay), heads(a)

    def step(S, inp):
        r_t, w_t, k_t, v_t, kk_t, a_t = inp
        sa = jnp.einsum('bhvk,bhk->bhv', S, -kk_t)
        S = (S * w_t[:, :, None, :]
             + sa[..., None] * (kk_t * a_t)[:, :, None, :]
             + v_t[..., None] * k_t[:, :, None, :])
        return S, jnp.einsum('bhvk,bhk->bhv', S, r_t)

    xs = tuple(jnp.moveaxis(t, 1, 0) for t in (r_h, w_h, k_h, v_h, kk, a_h))
    S0 = jnp.zeros((B, H, N, N), jnp.float32)
    _, ys = lax.scan(step, S0, xs)
    y = jnp.moveaxis(ys, 0, 1)
    mu = jnp.mean(y, axis=-1, keepdims=True)
    var = jnp.mean(jnp.square(y - mu), axis=-1, keepdims=True)
    y = ((y - mu) * lax.rsqrt(var + GN_EPS)).reshape(B, T, RWKV_DIM) * gn_g + gn_b
    bonus = jnp.sum(r_h * k_h * r_k, axis=-1, keepdims=True) * v_h
    y = y + bonus.reshape(B, T, RWKV_DIM)
    return (y * g).astype(p.dtype)


def partial_rope(t, pos):
    half = ROPE_DIM // 2
    inv_freq = jnp.power(ROPE_THETA, -jnp.arange(0, ROPE_DIM, 2, dtype=jnp.float32) / ROPE_DIM)
    ang = pos.astype(jnp.float32)[:, None] * inv_freq[None, :]
    cos = jnp.cos(ang)[None, :, None, :]
    sin = jnp.sin(ang)[None, :, None, :]
    tf = t[..., :ROPE_DIM].astype(jnp.float32)
    x1, x2 = tf[..., :half], tf[..., half:]
    rot = jnp.concatenate([x1 * cos - x2 * sin, x2 * cos + x1 * sin], axis=-1).astype(t.dtype)
    return jnp.concatenate([rot, t[..., ROPE_DIM:]], axis=-1)


def moba_attention(q, k, v):
    B, T, H, Dh = q.shape
    n_kblk = -(-T // MOBA_BLOCK)
    n_qblk = T // Q_BLOCK
    topk = min(MOBA_TOPK, n_kblk)
    pad = n_kblk * MOBA_BLOCK - T

    def to_blocks(t):
        t = jnp.pad(t, ((0, 0), (0, pad), (0, 0), (0, 0)))
        return t.reshape(B, n_kblk, MOBA_BLOCK, H, Dh).transpose(0, 3, 1, 2, 4)

    k_blocks = to_blocks(k)
    v_blocks = to_blocks(v)
    k_mean = jnp.mean(k_blocks.astype(jnp.float32), axis=3)
    q_blocks = q.reshape(B, n_qblk, Q_BLOCK, H, Dh).transpose(0, 1, 3, 2, 4).reshape(B * n_qblk, H, Q_BLOCK, Dh)
    b_ids = jnp.repeat(jnp.arange(B, dtype=jnp.int32), n_qblk)
    qb_ids = jnp.tile(jnp.arange(n_qblk, dtype=jnp.int32), B)
    scale = Dh ** -0.5
    head_ids = jnp.arange(H)[:, None, None]
    blk_ids = jnp.arange(n_kblk)

    def one_block(args):
        q_blk, b, qb = args
        kb, vb, km = k_blocks[b], v_blocks[b], k_mean[b]
        q_start = qb * Q_BLOCK
        own = q_start // MOBA_BLOCK
        gate = jnp.einsum('hqd,hnd->hqn', q_blk.astype(jnp.float32), km)
        gate = jnp.where(blk_ids < own, gate, NEG_INF)
        _, sel = lax.top_k(gate, topk)
        sel_valid = sel < own
        kg = kb[head_ids, sel]
        vg = vb[head_ids, sel]
        s_sel = jnp.einsum('hqd,hqnkd->hqnk', q_blk, kg, preferred_element_type=jnp.float32) * scale
        s_sel = jnp.where(sel_valid[..., None], s_sel, NEG_INF).reshape(H, Q_BLOCK, topk * MOBA_BLOCK)
        ko = lax.dynamic_index_in_dim(kb, own, axis=1, keepdims=False)
        vo = lax.dynamic_index_in_dim(vb, own, axis=1, keepdims=False)
        s_own = jnp.einsum('hqd,hkd->hqk', q_blk, ko, preferred_element_type=jnp.float32) * scale
        q_pos = q_start + jnp.arange(Q_BLOCK)
        k_pos = own * MOBA_BLOCK + jnp.arange(MOBA_BLOCK)
        s_own = jnp.where(k_pos[None, None, :] <= q_pos[None, :, None], s_own, NEG_INF)
        probs = jax.nn.softmax(jnp.concatenate([s_sel, s_own], axis=-1), axis=-1)
        p_sel = probs[..., :topk * MOBA_BLOCK].reshape(H, Q_BLOCK, topk, MOBA_BLOCK).astype(vb.dtype)
        p_own = probs[..., topk * MOBA_BLOCK:].astype(vb.dtype)
        return jnp.einsum('hqnk,hqnkd->hqd', p_sel, vg) + jnp.einsum('hqk,hkd->hqd', p_own, vo)

    out = lax.map(one_block, (q_blocks, b_ids, qb_ids))
    return out.reshape(B, n_qblk, H, Q_BLOCK, Dh).transpose(0, 1, 3, 2, 4).reshape(B, T, H * Dh)


def token_mixer(x, w_in, shift_mix, decay_w0, decay_up, iclr_a0, iclr_up, gate_up, k_k, k_a, r_k,
                gn_g, gn_b, w_branch_rwkv, w_branch_attn, w_out):
    B, T, _ = x.shape
    proj = jnp.einsum('btd,dc->btc', x, w_in)
    p_rwkv = proj[..., :RWKV_COLS]
    p_attn = proj[..., RWKV_COLS:RWKV_COLS + ATTN_COLS]
    p_gate = proj[..., RWKV_COLS + ATTN_COLS:]
    y_rwkv = rwkv7_time_mix(p_rwkv, shift_mix, decay_w0, decay_up, iclr_a0, iclr_up, gate_up,
                            k_k, k_a, r_k, gn_g, gn_b) @ w_branch_rwkv
    q, k, v = jnp.split(p_attn, 3, axis=-1)
    q, k, v = (t.reshape(B, T, ATTN_HEADS, ATTN_HEAD_DIM) for t in (q, k, v))
    pos = jnp.arange(T)
    q, k = partial_rope(q, pos), partial_rope(k, pos)
    y_attn = moba_attention(q, k, v) @ w_branch_attn
    gates = jax.nn.sigmoid(p_gate).reshape(B, T, N_BRANCHES, D_MODEL)
    merged = gates[:, :, 0] * y_rwkv + gates[:, :, 1] * y_attn
    return merged @ w_out


def moe_ffn(x, router_w, router_b, expert_w_in, expert_b_in, expert_w_out, expert_b_out):
    B, T, D = x.shape
    xf = x.reshape(B * T, D)
    n_tok = B * T
    logits = (xf @ router_w + router_b).astype(jnp.float32)
    top_val, top_idx = lax.top_k(logits, TOP_K)
    weights = jax.nn.softmax(top_val, axis=-1)
    n_assign = n_tok * TOP_K
    flat_e = top_idx.reshape(-1)
    flat_tok = jnp.repeat(jnp.arange(n_tok, dtype=jnp.int32), TOP_K)
    flat_w = weights.reshape(-1)
    order = jnp.argsort(flat_e)
    sorted_e, sorted_tok, sorted_w = flat_e[order], flat_tok[order], flat_w[order]
    counts = jnp.bincount(flat_e, length=N_EXPERTS)
    starts = jnp.cumsum(counts) - counts
    padded = (counts + EXPERT_ROW_BLOCK - 1) // EXPERT_ROW_BLOCK * EXPERT_ROW_BLOCK
    p_ends = jnp.cumsum(padded)
    p_starts = p_ends - padded
    dest = p_starts[sorted_e] + jnp.arange(n_assign) - starts[sorted_e]
    n_rblk = -(-n_assign // EXPERT_ROW_BLOCK) + N_EXPERTS
    n_rows = n_rblk * EXPERT_ROW_BLOCK
    row_tok = jnp.zeros((n_rows,), jnp.int32).at[dest].set(sorted_tok)
    row_w = jnp.zeros((n_rows,), jnp.float32).at[dest].set(sorted_w)
    blk_start = jnp.arange(n_rblk) * EXPERT_ROW_BLOCK
    blk_e = jnp.minimum(jnp.sum(blk_start[:, None] >= p_ends[None, :], axis=1), N_EXPERTS - 1)

    def expert_block(args):
        toks, e = args
        h = xf[toks] @ expert_w_in[e] + expert_b_in[e]
        gate_h = jnp.minimum(h[:, :D_EXPERT], SWIGLU_LIMIT)
        lin_h = jnp.clip(h[:, D_EXPERT:], -SWIGLU_LIMIT, SWIGLU_LIMIT)
        act = gate_h * jax.nn.sigmoid(SWIGLU_ALPHA * gate_h) * (lin_h + 1.0)
        return act @ expert_w_out[e] + expert_b_out[e]

    ys = lax.map(expert_block, (row_tok.reshape(n_rblk, EXPERT_ROW_BLOCK), blk_e)).reshape(n_rows, D)
    out = jax.ops.segment_sum(ys * row_w[:, None].astype(ys.dtype), row_tok, num_segments=n_tok)
    return out.reshape(B, T, D).astype(x.dtype)


def setup_inputs(seed: int = 0) -> dict:
    key = jax.random.key(seed)
    ks = jax.random.split(key, 26)
    L, D, E, F = DEPTH, D_MODEL, N_EXPERTS, D_EXPERT
    nrm = lambda k, shape, s: jax.random.normal(k, shape, jnp.float32) * s
    return {
        'x': jax.random.normal(ks[0], (BATCH, SEQ, D), jnp.float32),
        'ln1_g': 1.0 + nrm(ks[1], (L, D), 0.01),
        'ln1_b': nrm(ks[2], (L, D), 0.01),
        'ln2_g': 1.0 + nrm(ks[3], (L, D), 0.01),
        'ln2_b': nrm(ks[4], (L, D), 0.01),
        'w_in': nrm(ks[5], (L, D, IN_COLS), D ** -0.5),
        'shift_mix': jax.random.uniform(ks[6], (L, RWKV_COLS), jnp.float32),
        'decay_w0': jax.random.uniform(ks[7], (L, RWKV_DIM), jnp.float32, -6.5, -1.5),
        'decay_up': nrm(ks[8], (L, DECAY_RANK, RWKV_DIM), 0.1 * DECAY_RANK ** -0.5),
        'iclr_a0': nrm(ks[9], (L, RWKV_DIM), 0.1),
        'iclr_up': nrm(ks[10], (L, ICLR_RANK, RWKV_DIM), 0.1 * ICLR_RANK ** -0.5),
        'gate_up': nrm(ks[11], (L, GATE_RANK, RWKV_DIM), GATE_RANK ** -0.5),
        'k_k': 0.85 + nrm(ks[12], (L, RWKV_DIM), 0.05),
        'k_a': 1.0 + nrm(ks[13], (L, RWKV_DIM), 0.05),
        'r_k': nrm(ks[14], (L, RWKV_HEADS, RWKV_HEAD_DIM), 0.1),
        'gn_g': 1.0 + nrm(ks[15], (L, RWKV_DIM), 0.01),
        'gn_b': nrm(ks[16], (L, RWKV_DIM), 0.01),
        'w_branch_rwkv': nrm(ks[17], (L, RWKV_DIM, D), RWKV_DIM ** -0.5),
        'w_branch_attn': nrm(ks[18], (L, ATTN_DIM, D), ATTN_DIM ** -0.5),
        'w_out': nrm(ks[19], (L, D, D), DEEPNORM_BETA * D ** -0.5),
        'router_w': nrm(ks[20], (L, D, E), D ** -0.5),
        'router_b': nrm(ks[21], (L, E), 0.01),
        'expert_w_in': nrm(ks[22], (L, E, D, 2 * F), D ** -0.5),
        'expert_b_in': nrm(ks[23], (L, E, 2 * F), 0.01),
        'expert_w_out': nrm(ks[24], (L, E, F, D), DEEPNORM_BETA * F ** -0.5),
        'expert_b_out': nrm(ks[25], (L, E, D), 0.01),
    }


def reference(x, ln1_g, ln1_b, ln2_g, ln2_b, w_in, shift_mix, decay_w0, decay_up, iclr_a0, iclr_up,
              gate_up, k_k, k_a, r_k, gn_g, gn_b, w_branch_rwkv, w_branch_attn, w_out,
              router_w, router_b, expert_w_in, expert_b_in, expert_w_out, expert_b_out):
    for i in range(DEPTH):
        mix = token_mixer(x, w_in[i], shift_mix[i], decay_w0[i], decay_up[i], iclr_a0[i], iclr_up[i],
                          gate_up[i], k_k[i], k_a[i], r_k[i], gn_g[i], gn_b[i],
                          w_branch_rwkv[i], w_branch_attn[i], w_out[i])
        x = layer_norm(DEEPNORM_ALPHA * x + mix, ln1_g[i], ln1_b[i])
        ffn = moe_ffn(x, router_w[i], router_b[i], expert_w_in[i], expert_b_in[i],
                      expert_w_out[i], expert_b_out[i])
        x = layer_norm(DEEPNORM_ALPHA * x + ffn, ln2_g[i], ln2_b[i])
    return x
```

```python
from contextlib import ExitStack
import numpy as np
import concourse.bass as bass
import concourse.mybir as mybir
from concourse.bass_utils import run_bass_kernel_spmd

F32 = mybir.dt.float32
BF16 = mybir.dt.bfloat16
I32 = mybir.dt.int32
ALU = mybir.AluOpType
AF = mybir.ActivationFunctionType
AX = mybir.AxisListType

D = 1024
ALPHA = 2.0 ** 0.25
C0 = float(np.exp(-0.5))
GN_EPS = 64e-5
LN_EPS = 1e-5
ENGS = ("pe", "act", "dve", "pool", "sp")


class Buf:
    __slots__ = ("name", "lw", "rd")

    def __init__(self, name):
        self.name = name
        self.lw = None
        self.rd = []


class Tl:
    __slots__ = ("ap", "b")

    def __init__(self, ap, b):
        self.ap = ap
        self.b = b


class Prog:
    NSLOT = 8

    def __init__(self, nc):
        self.nc = nc
        self.q = {e: [] for e in ENGS}
        self.cnt = {e: 0 for e in ENGS}
        self.seen = {e: {} for e in ENGS}
        self.dma_i = {"sp": 0, "pool": 0, "act": 0}
        self.dma_uses = {}
        self.nbuf = 0
        self.phase = None
        self.scopes = False

    def buf(self, name=None):
        self.nbuf += 1
        return Buf(name or f"b{self.nbuf}")

    def _waits(self, eng, reads, writes):
        w = {}

        def add(ev):
            if ev is None:
                return
            k, v = ev
            if eng == "pe" and k == "pe":
                return
            if w.get(k, 0) < v:
                w[k] = v

        for b in reads:
            add(b.lw)
        for b in writes:
            add(b.lw)
            for ev in b.rd:
                add(ev)
        return w

    def _commit(self, eng, w, reads, writes, ev):
        seen = self.seen[eng]
        out = []
        for k, v in w.items():
            if seen.get(k, 0) < v:
                seen[k] = v
                out.append((k, v))
        for b in reads:
            b.rd.append(ev)
            if len(b.rd) > 64:
                best = {}
                for k, v in b.rd:
                    if best.get(k, 0) < v:
                        best[k] = v
                b.rd = list(best.items())
        for b in writes:
            b.lw = ev
            b.rd = []
        return out

    def op(self, eng, fn, reads=(), writes=()):
        w = self._waits(eng, reads, writes)
        self.cnt[eng] += 1
        ev = (eng, self.cnt[eng])
        waits = self._commit(eng, w, reads, writes, ev)
        self.q[eng].append((waits, fn, (eng, 1), self.phase))
        return ev

    def dma(self, fn, reads=(), writes=(), eng="sp"):
        w = self._waits(eng, reads, writes)
        i = self.dma_i[eng]
        self.dma_i[eng] += 1
        slot = f"d_{eng}_{i % self.NSLOT}"
        uses = self.dma_uses.get(slot, 0)
        if uses > 0 and w.get(slot, 0) < 16 * uses:
            w[slot] = 16 * uses
        self.dma_uses[slot] = uses + 1
        ev = (slot, 16 * (uses + 1))
        waits = self._commit(eng, w, reads, writes, ev)
        self.q[eng].append((waits, fn, (slot, 16), self.phase))
        return ev

    def barrier(self):
        evs = [(e, self.cnt[e]) for e in ENGS if self.cnt[e] > 0]
        evs += [(s, 16 * u) for s, u in self.dma_uses.items()]
        for e in ENGS:
            waits = []
            for k, v in evs:
                if e == "pe" and k == "pe":
                    continue
                if self.seen[e].get(k, 0) < v:
                    self.seen[e][k] = v
                    waits.append((k, v))
            self.q[e].append((waits, None, None, None))

    def emit(self):
        nc = self.nc
        names = set()
        for e in ENGS:
            for waits, fn, inc, _ph in self.q[e]:
                for k, _ in waits:
                    names.add(k)
                if inc is not None:
                    names.add(inc[0])
        with ExitStack() as st:
            sems = {n: st.enter_context(nc.semaphore("s_" + n)) for n in sorted(names)}
            block = st.enter_context(nc.Block())
            q = self.q

            def run(eng_obj, ops):
                for waits, fn, inc, ph in ops:
                    for k, v in waits:
                        eng_obj.wait_ge(sems[k], v)
                    if fn is not None:
                        if self.scopes and ph is not None:
                            with nc.named_scope(ph):
                                fn(eng_obj).then_inc(sems[inc[0]], inc[1])
                        else:
                            fn(eng_obj).then_inc(sems[inc[0]], inc[1])

            @block.tensor
            def _(e):
                run(e, q["pe"])

            @block.scalar
            def _(e):
                run(e, q["act"])

            @block.vector
            def _(e):
                run(e, q["dve"])

            @block.gpsimd
            def _(e):
                run(e, q["pool"])

            @block.sync
            def _(e):
                run(e, q["sp"])


class Arena:
    def __init__(self, ap, P, words):
        self.ap, self.P, self.off, self.words = ap, P, 0, words

    def f32(self, n, name=None):
        n2 = (n + 1) // 2 * 2
        assert self.off + n2 <= self.words, f"arena overflow {name} {self.off}+{n2}>{self.words}"
        v = self.ap[:, self.off:self.off + n]
        self.off += n2
        return Tl(v, self.P.buf(name))

    def bf(self, n, name=None):
        w = (n + 1) // 2
        t = self.f32(w, name)
        return Tl(t.ap.bitcast(BF16)[:, 0:n], t.b)

    def i32(self, n, name=None):
        t = self.f32(n, name)
        return Tl(t.ap.bitcast(I32), t.b)


def cap_for(T):
    if T == 4096:
        return 640
    return max(256, ((T // 8) * 3 // 2 + 127) // 128 * 128)


def host_consts(T):
    c = {}
    c["identF"] = np.eye(128, dtype=np.float32)
    bo = np.zeros((128, 128), np.float32)
    bo[:64, :64] = 1.0
    bo[64:, 64:] = 1.0
    c["bones"] = bo
    p = np.arange(128)[:, None]
    q = np.arange(128)[None, :]
    su = (p < q).astype(np.float32)
    sl = (q < p).astype(np.float32)
    sui = (p <= q).astype(np.float32)
    c["mskA"] = np.concatenate([su, su, sl, sl], 1)
    c["mskB"] = np.concatenate([su, su, sui, sui], 1)
    c["striu"] = su.copy()
    half = 8
    inv = np.power(500000.0, -np.arange(0, 16, 2, dtype=np.float32) / 16.0).astype(np.float32)
    ang = np.arange(T, dtype=np.float32)[None, :] * inv[:, None]
    cosv, sinv = np.cos(ang).astype(np.float32), np.sin(ang).astype(np.float32)
    Ct = np.ones((128, T), np.float32)
    St = np.zeros((128, T), np.float32)
    Rm = np.zeros((128, 128), np.float32)
    for h in range(2):
        b0 = 64 * h
        Ct[b0:b0 + 8] = cosv
        Ct[b0 + 8:b0 + 16] = cosv
        St[b0:b0 + 8] = sinv
        St[b0 + 8:b0 + 16] = sinv
        for i in range(8):
            Rm[b0 + i, b0 + i + 8] = -1.0
            Rm[b0 + i + 8, b0 + i] = 1.0
    c["ropeC"], c["ropeS"] = Ct, St
    c["RmT"] = np.ascontiguousarray(Rm.T)
    cm = np.zeros((4, 128, 512), np.float32)
    tri = (p <= q).astype(np.float32)
    for kl in range(4):
        for qs in range(4):
            blk_k, blk_q = kl // 2, qs // 2
            if blk_k < blk_q:
                m = 1.0
            elif blk_k > blk_q:
                m = 0.0
            elif kl < qs:
                m = 1.0
            elif kl > qs:
                m = 0.0
            else:
                m = tri
            cm[kl, :, qs * 128:(qs + 1) * 128] = m
    c["cmask"] = cm.transpose(1, 0, 2).reshape(128, 2048).copy()
    c["ecap"] = np.tile((np.arange(32, dtype=np.float32) * cap_for(T))[None, :], (128, 1))
    return c


class _Done(Exception):
    pass


def build(T, stage=2, stop=0, scopes=False):
    try:
        return _build(T, stage, stop, scopes)
    except _Done as d:
        return d.args[0]


def _build(T, stage=2, stop=0, scopes=False):
    NT, NS = T // 512, T // 128
    CAP = cap_for(T)
    NBLK = CAP // 128
    HALF = CAP // 2
    NROW = 32 * CAP
    nc = bass.Bass("TRN2", target_bir_lowering=False)

    def din(name, shape, dt=F32):
        return nc.dram_tensor(name, list(shape), dt, kind="ExternalInput").ap()

    x_d = din("x", [T, D])
    w_in_d = din("w_in", [D, 5376])
    vec_d = din("vec", [128, 42])
    dup_d = din("dup", [128, 512])
    gup_d = din("gate_up", [128, 512])
    wbr_d = din("w_branch_rwkv", [512, D])
    wba_d = din("w_branch_attn", [512, D])
    wo_d = din("w_out", [D, D])
    lnp_d = din("lnp", [4, 128, D])
    rw_d = din("router_w", [D, 32])
    rb_d = din("router_b_bc", [128, 32])
    ewin_d = din("expert_w_in", [32, D, 2048])
    ebin_d = din("expert_b_in_fm", [128, 32 * 16])
    ewout_d = din("expert_w_out", [32, D, D])
    ebout_d = din("expert_b_out", [32, D])
    cst = host_consts(T)
    cd = {k: din("c_" + k, v.shape) for k, v in cst.items()}
    out_d = nc.dram_tensor("out", [T, D], F32, kind="ExternalOutput").ap()
    X1_d = nc.dram_tensor("X1s", [T, D], F32, kind="Internal").ap()
    XE_d = nc.dram_tensor("XEs", [NROW + 1, D], BF16, kind="Internal").ap()
    Y_d = nc.dram_tensor("Ys", [NROW + 1, D], F32, kind="Internal").ap()
    KT_d = nc.dram_tensor("KTs", [4, 128, T], BF16, kind="Internal").ap()
    Wb_d = nc.dram_tensor("Wbs", [42, 128, 1024], BF16, kind="Internal").ap()
    V_d = nc.dram_tensor("Vs", [NS, 128, 8 * 66], BF16, kind="Internal").ap()

    P = Prog(nc)
    P.scopes = scopes
    WORDS = 51200
    with ExitStack() as st:
        arena_t = st.enter_context(nc.sbuf_tensor("arena", [128, WORDS], F32))
        A = Arena(arena_t[:, :], P, WORDS)
        psb = [Tl(st.enter_context(nc.psum_tensor(f"ps{i}", [128, 512], F32))[:, :], P.buf(f"ps{i}")) for i in range(8)]
        psi = [0]

        def PS():
            t = psb[psi[0] % 7]
            psi[0] += 1
            return t

        Bout = P.buf("out")
        BX1, BXE, BY = P.buf("X1"), P.buf("XE"), P.buf("Y")
        BKT, BV = P.buf("KTd"), P.buf("Vd")
        BWB = [P.buf(f"Wb{c}") for c in range(42)]

        def bl(ts):
            return [t.b if isinstance(t, Tl) else t for t in ts]

        def MM(out, lhsT, rhs, R, W, start=True, stop=True, tp=None):
            kw = {} if tp is None else {"tile_position": tp}
            P.op("pe", lambda e: e.matmul(out, lhsT, rhs, start=start, stop=stop, **kw), bl(R), bl(W))

        def TR(out, in_, ident, R, W):
            P.op("pe", lambda e: e.transpose(out, in_, ident), bl(R), bl(W))

        def ACT(out, in_, func, R, W, bias=None, scale=None, accum=None):
            kw = {}
            if bias is not None:
                kw["bias"] = bias
            if scale is not None:
                kw["scale"] = scale
            if accum is not None:
                kw["accum_out"] = accum
            P.op("act", lambda e: e.activation(out=out, in_=in_, func=func, **kw), bl(R), bl(W))

        def TT(out, a, b, op, R, W, eng="dve"):
            P.op(eng, lambda e: e.tensor_tensor(out=out, in0=a, in1=b, op=op), bl(R), bl(W))

        def TS(out, a, s1, s2, op0, op1, R, W, eng="dve", accum=None):
            if op1 is None:
                P.op(eng, lambda e: e.tensor_scalar(out, a, s1, None, op0), bl(R), bl(W))
            elif accum is None:
                P.op(eng, lambda e: e.tensor_scalar(out, a, s1, s2, op0, op1), bl(R), bl(W))
            else:
                P.op(eng, lambda e: e.tensor_scalar(out, a, s1, s2, op0, op1, accum), bl(R), bl(W))

        def STT(out, in0, scalar, in1, op0, op1, R, W, accum=None):
            if accum is None:
                P.op("dve", lambda e: e.scalar_tensor_tensor(out=out, in0=in0, scalar=scalar, in1=in1, op0=op0, op1=op1), bl(R), bl(W))
            else:
                P.op("dve", lambda e: e.scalar_tensor_tensor(out=out, in0=in0, scalar=scalar, in1=in1, op0=op0, op1=op1, accum_out=accum), bl(R), bl(W))

        def CP(out, in_, R, W, eng="dve"):
            if eng == "act":
                P.op("act", lambda e: e.activation(out=out, in_=in_, func=AF.Copy), bl(R), bl(W))
            else:
                P.op(eng, lambda e: e.tensor_copy(out, in_), bl(R), bl(W))

        def RCP(out, in_, R, W):
            P.op("dve", lambda e: e.reciprocal(out, in_), bl(R), bl(W))

        def MSET(ap, val, W, eng="pool"):
            P.op(eng, lambda e: e.memset(ap, val), [], bl(W))

        _bc = {}

        def BC(e):
            if "r" not in _bc:
                _bc["r"] = e.to_reg(NROW)
            return _bc["r"]

        def DMA(out, in_, R, W, eng="sp"):
            P.dma(lambda e: e.dma_start(out=out, in_=in_), bl(R), bl(W), eng=eng)

        def CK(k, ap=None, R=()):
            if stop != k:
                return
            if ap is not None:
                n = ap.shape[-1]
                DMA(out_d[0:ap.shape[0], 0:n], ap, list(R), [Bout])
            P.barrier()
            P.emit()
            raise _Done(nc)

        identF = A.f32(128, "identF")
        DMA(identF.ap, cd["identF"], [], [identF])
        identB = A.bf(128, "identB")
        CP(identB.ap, identF.ap, [identF], [identB])
        onesF = A.f32(128, "onesF")
        MSET(onesF.ap, 1.0, [onesF])
        lnp = [A.f32(D, f"lnp{i}") for i in range(2)]
        for i in range(2):
            DMA(lnp[i].ap, lnp_d[i], [], [lnp[i]])
        Wk = A.f32(NS * 4, "Wk")
        IDX = A.i32(NS * 4, "IDX")
        Wk3 = Wk.ap.rearrange("p (s k) -> p s k", k=4)
        IDX3 = IDX.ap.rearrange("p (s k) -> p s k", k=4)
        mark_all = A.off

        bones = A.f32(128, "bones")
        DMA(bones.ap, cd["bones"], [], [bones])
        bo64 = A.f32(128, "bo64")
        TS(bo64.ap, bones.ap, 1.0 / 64.0, None, ALU.mult, None, [bones], [bo64])
        mskA = A.f32(512, "mskA")
        DMA(mskA.ap, cd["mskA"], [], [mskA])
        mskB = A.f32(512, "mskB")
        DMA(mskB.ap, cd["mskB"], [], [mskB])
        striu = A.f32(128, "striu")
        DMA(striu.ap, cd["striu"], [], [striu])
        RmT = A.f32(128, "RmT")
        DMA(RmT.ap, cd["RmT"], [], [RmT])
        cmask = A.bf(2048, "cmask")
        ecap = A.f32(32, "ecap")
        DMA(ecap.ap, cd["ecap"], [], [ecap])
        vec = A.f32(42, "vec")
        DMA(vec.ap, vec_d, [], [vec])
        omm = A.f32(14, "omm")
        TS(omm.ap, vec.ap[:, 0:14], -1.0, 1.0, ALU.mult, ALU.add, [vec], [omm])
        V_W0, V_A0, V_KK, V_KA, V_RK, V_GG, V_GB = 14, 18, 22, 26, 30, 34, 38
        dup = A.f32(512, "dup")
        DMA(dup.ap, dup_d, [], [dup])
        gup = A.f32(512, "gup")
        DMA(gup.ap, gup_d, [], [gup])
        rw = A.f32(8 * 32, "rw")
        rw3 = rw.ap.rearrange("p (k e) -> p k e", k=8)
        DMA(rw3, rw_d.rearrange("(k p) e -> p k e", p=128), [], [rw])
        rb = A.f32(32, "rb")
        DMA(rb.ap, rb_d, [], [rb])
        Wbr = A.bf(4 * D, "Wbr")
        Wbr3 = Wbr.ap.rearrange("p (k c) -> p k c", k=4)
        DMA(Wbr3, wbr_d.rearrange("(k p) c -> p k c", p=128), [], [Wbr], eng="pool")
        Wba = A.bf(4 * D, "Wba")
        Wba3 = Wba.ap.rearrange("p (k c) -> p k c", k=4)
        DMA(Wba3, wba_d.rearrange("(k p) c -> p k c", p=128), [], [Wba], eng="pool")
        Wo = A.bf(8 * D, "Wo")
        Wo3 = Wo.ap.rearrange("p (k c) -> p k c", k=8)
        DMA(Wo3, wo_d.rearrange("(k p) c -> p k c", p=128), [], [Wo], eng="pool")
        km = [A.f32(16, f"km{p}") for p in range(4)]
        for p_ in range(4):
            MSET(km[p_].ap, 0.0, [km[p_]])
        Hs = [A.f32(128, f"H{p}") for p in range(4)]
        for p_ in range(4):
            MSET(Hs[p_].ap, 0.0, [Hs[p_]])
        carry = A.f32(14, "carry")
        MSET(carry.ap, 0.0, [carry])
        cnt = A.f32(32, "cnt")
        MSET(cnt.ap, 0.0, [cnt])
        jidx = A.f32(128, "jidx")
        P.op("pool", lambda e: e.iota(jidx.ap.rearrange("p (h j) -> p h j", h=8), pattern=[[0, 8], [1, 16]], base=0,
                                      channel_multiplier=0, allow_small_or_imprecise_dtypes=True), [], [jidx.b])
        xT = A.bf(8 * 512, "xT")
        xT3 = xT.ap.rearrange("p (k t) -> p k t", k=8)
        wring = [A.bf(8 * 128, f"wr{i}") for i in range(6)]
        wri = [0]
        xs_t = [A.f32(D, f"xs{i}") for i in range(2)]
        ropeC = A.f32(512, "ropeC")
        ropeS = A.f32(512, "ropeS")
        NSCR = 24
        scr = [A.f32(514, f"scr{i}") for i in range(NSCR)]
        free = list(range(NSCR))

        def b16(t):
            return t.ap.bitcast(BF16)[:, 0:512]

        def G():
            assert free, "scratch pool exhausted"
            return scr[free.pop(0)]

        def F(*ts):
            for t in ts:
                free.append(scr.index(t))

        yrT = A.bf(4 * 512, "yrT")
        yrT3 = yrT.ap.rearrange("p (k t) -> p k t", k=4)
        aoT = A.bf(4 * 512, "aoT")
        aoT3 = aoT.ap.rearrange("p (k t) -> p k t", k=4)
        mg = A.bf(8 * 512, "mg")
        mg3 = mg.ap.rearrange("p (k t) -> p k t", k=8)
        reg0 = A.off
        KTp = A.bf(T, "KTp")
        Vp = A.bf(NS * 2 * 66, "Vp")
        Vp4 = Vp.ap.rearrange("p (s h d) -> p s h d", s=NS, h=2)
        kst = A.bf(512, "kst")
        vst = A.bf(4 * 8 * 66, "vst")
        vst4 = vst.ap.rearrange("p (s h d) -> p s h d", s=4, h=8)
        QT = A.bf(4 * 512, "QT")
        QT3 = QT.ap.rearrange("p (k t) -> p k t", k=4)
        selm = A.f32(4 * 128, "selm")
        selm4 = selm.ap.rearrange("p (q h j) -> p q h j", q=4, h=8)
        acc = A.f32(2 * 4 * 66, "acc")
        acc4 = acc.ap.rearrange("p (h q d) -> p h q d", h=2, q=4)
        PTb = [A.bf(512, f"PT{i}") for i in range(8)]
        reg_att = A.off
        A.off = reg0
        CH = []
        for c4 in range(4):
            CH.append(dict(tok=A.bf(512, f"c_tok{c4}"), M2=A.bf(512, f"c_M2{c4}"), M3=A.bf(256, f"c_M3{c4}"),
                           Wa=A.bf(256, f"c_Wa{c4}"), Wb=A.bf(256, f"c_Wb{c4}"), NL=A.bf(512, f"c_NL{c4}"),
                           NL2=A.bf(512, f"c_NL2{c4}"), AbT=A.bf(128, f"c_AbT{c4}")))
        A.off = max(A.off, reg_att)
        x1b = A.bf(D, "x1b")
        for q4 in range(4):
            tq = G()
            DMA(tq.ap[:, 0:512], cd["cmask"][:, q4 * 512:(q4 + 1) * 512], [], [tq])
            CP(cmask.ap[:, q4 * 512:(q4 + 1) * 512], tq.ap[:, 0:512], [tq], [cmask])
            F(tq)
        sm = A.f32(256, "sm")
        print("stage A arena words", A.off)
        MSET(x1b.ap, 0.0, [x1b])
        zsrc = x1b.ap.rearrange("p (o c) -> p o c", o=1).to_broadcast([128, NBLK, D])
        for e_ in range(32):
            DMA(XE_d[e_ * CAP:(e_ + 1) * CAP, :].rearrange("(b p) c -> p b c", p=128), zsrc, [x1b], [BXE])
        DMA(XE_d[NROW:NROW + 1, :], x1b.ap[0:1, :], [x1b], [BXE])
        MSET(xs_t[0].ap, 0.0, [xs_t[0]])
        DMA(Y_d[NROW:NROW + 1, :], xs_t[0].ap[0:1, :], [xs_t[0]], [BY])

        for c in range(42):
            t = wring[c % len(wring)]
            v = t.ap.rearrange("p (k c) -> p k c", k=8)
            DMA(v, w_in_d.rearrange("(k p) c -> p k c", p=128)[:, :, c * 128:(c + 1) * 128], [], [t], eng="pool")
            DMA(Wb_d[c], t.ap, [t], [BWB[c]])

        def wchunk(c):
            t = wring[wri[0] % len(wring)]
            wri[0] += 1
            v = t.ap.rearrange("p (k c) -> p k c", k=8)
            DMA(t.ap, Wb_d[c], [BWB[c]], [t])
            return t, v

        def proj(c, ps):
            t, v = wchunk(c)
            for kc in range(8):
                MM(ps.ap, v[:, kc, :], xT3[:, kc, :], [t, xT], [ps], start=(kc == 0), stop=(kc == 7))

        def proj_shift(c):
            ps = PS()
            proj(c, ps)
            raw = G()
            CP(raw.ap[:, 0:1], carry.ap[:, c:c + 1], [carry], [raw], eng="pool")
            CP(raw.ap[:, 1:513], ps.ap, [ps], [raw], eng="act")
            CP(carry.ap[:, c:c + 1], raw.ap[:, 512:513], [raw], [carry], eng="pool")
            tmp = G()
            TS(tmp.ap[:, 0:512], raw.ap[:, 1:513], omm.ap[:, c:c + 1], None, ALU.mult, None, [raw, omm], [tmp])
            o = G()
            STT(o.ap[:, 0:512], raw.ap[:, 0:512], vec.ap[:, c:c + 1], tmp.ap[:, 0:512], ALU.mult, ALU.add, [raw, vec, tmp], [o])
            F(raw, tmp)
            return o

        def proj_shift_multi(chunks):
            pss = []
            for c in chunks:
                ps = PS()
                proj(c, ps)
                pss.append(ps)
            raws = []
            for c, ps in zip(chunks, pss):
                raw = G()
                CP(raw.ap[:, 0:1], carry.ap[:, c:c + 1], [carry], [raw], eng="pool")
                CP(raw.ap[:, 1:513], ps.ap, [ps], [raw], eng="act")
                raws.append(raw)
            for c, raw in zip(chunks, raws):
                CP(carry.ap[:, c:c + 1], raw.ap[:, 512:513], [raw], [carry], eng="pool")
            tmps = []
            for c, raw in zip(chunks, raws):
                tmp = G()
                TS(tmp.ap[:, 0:512], raw.ap[:, 1:513], omm.ap[:, c:c + 1], None, ALU.mult, None, [raw, omm], [tmp])
                tmps.append(tmp)
            outs = []
            for c, raw, tmp in zip(chunks, raws, tmps):
                o = G()
                STT(o.ap[:, 0:512], raw.ap[:, 0:512], vec.ap[:, c:c + 1], tmp.ap[:, 0:512], ALU.mult, ALU.add, [raw, vec, tmp], [o])
                outs.append(o)
            F(*raws, *tmps)
            return outs

        def ln_route(it, t0):
            P.phase = f"ln_route_t{it}"
            for qs in range(4):
                s = 4 * it + qs
                tsl = slice(t0 + qs * 128, t0 + (qs + 1) * 128)
                xs = xs_t[qs % 2]
                DMA(xs.ap, x_d[tsl, :], [], [xs])
                hpre = [G(), G()]
                x1t = [G(), G()]
                x1T = [G(), G()]
                for half in range(2):
                    ps = PS()
                    for kc in range(8):
                        MM(ps.ap, mg3[:, kc, qs * 128:(qs + 1) * 128], Wo3[:, kc, half * 512:(half + 1) * 512], [mg, Wo], [ps],
                           start=(kc == 0), stop=(kc == 7))
                    STT(hpre[half].ap[:, 0:512], xs.ap[:, half * 512:(half + 1) * 512], ALPHA, ps.ap, ALU.mult, ALU.add, [xs, ps], [hpre[half]])
                    yield
                layer_norm(P, TS, TT, ACT, RCP, hpre, x1t, lnp[0], lnp[1], sm)
                yield
                for half in range(2):
                    DMA(X1_d[tsl, half * 512:(half + 1) * 512], x1t[half].ap[:, 0:512], [x1t[half]], [BX1])
                    CP(x1b.ap[:, half * 512:(half + 1) * 512], x1t[half].ap[:, 0:512], [x1t[half]], [x1b], eng="act")
                for g4 in range(2):
                    ps = PS()
                    for j in range(4):
                        TR(ps.ap[:, j * 128:(j + 1) * 128], x1t[g4].ap[:, j * 128:(j + 1) * 128], identF.ap, [x1t[g4], identF], [ps])
                    CP(x1T[g4].ap[:, 0:512], ps.ap, [ps], [x1T[g4]], eng=("act" if g4 else "dve"))
                    yield
                CK(9, x1t[0].ap[:, 0:512], [x1t[0]])
                psl = PS()
                for kc in range(8):
                    MM(psl.ap[:, 0:32], x1T[kc // 4].ap[:, (kc % 4) * 128:(kc % 4 + 1) * 128], rw3[:, kc, :], [x1T[kc // 4], rw], [psl],
                       start=(kc == 0), stop=(kc == 7))
                F(*hpre, *x1T)
                lg = sm.ap[:, 16:48]
                TT(lg, psl.ap[:, 0:32], rb.ap, ALU.add, [psl, rb], [sm])
                yield
                m8 = sm.ap[:, 48:56]
                P.op("dve", lambda e, o=m8, i_=lg: e.max(out=o, in_=i_), [sm.b], [sm.b])
                nv0 = sm.ap[:, 56:57]
                TS(nv0, m8[:, 0:1], -1.0, None, ALU.mult, None, [sm], [sm])
                ev = sm.ap[:, 58:62]
                esum = sm.ap[:, 62:63]
                ACT(ev, m8[:, 0:4], AF.Exp, [sm], [sm], bias=nv0, accum=esum)
                RCP(esum, esum, [sm], [sm])
                TS(Wk3[:, s, :], ev, esum, None, ALU.mult, None, [sm], [Wk])
                yield
                mask = sm.ap[:, 64:96]
                TS(mask, lg, m8[:, 3:4], None, ALU.is_ge, None, [sm], [sm])
                psp = PS()
                MM(psp.ap[:, 0:32], striu.ap, mask, [striu, sm], [psp])
                MM(psp.ap[:, 32:64], onesF.ap, mask, [onesF, sm], [psp])
                slot = sm.ap[:, 96:128]
                ovf = sm.ap[:, 164:196]
                TT(slot, psp.ap[:, 0:32], cnt.ap, ALU.add, [psp, cnt], [sm])
                TS(ovf, slot, float(CAP), None, ALU.is_ge, None, [sm], [sm])
                TT(slot, slot, ecap.ap, ALU.add, [sm, ecap], [sm])
                TS(junk2 := sm.ap[:, 196:228], ovf, -1.0, 1.0, ALU.mult, ALU.add, [sm], [sm])
                TT(slot, slot, junk2, ALU.mult, [sm], [sm])
                STT(slot, ovf, float(NROW), slot, ALU.mult, ALU.add, [sm], [sm])
                TT(cnt.ap, cnt.ap, psp.ap[:, 32:64], ALU.add, [cnt, psp], [cnt])
                yield
                idxf = sm.ap[:, 128:132]
                junk = sm.ap[:, 132:164]
                for k in range(4):
                    STT(junk, lg, m8[:, k:k + 1], slot, ALU.is_equal, ALU.mult, [sm], [sm], accum=idxf[:, k:k + 1])
                CP(IDX3[:, s, :], idxf, [sm], [IDX])
                CK(10, sm.ap[:, 0:164], [sm])
                yield
                for k in range(4):
                    P.dma(lambda e, o=IDX3[:, s, k:k + 1]: e.indirect_dma_start(
                        out=XE_d, out_offset=bass.IndirectOffsetOnAxis(ap=o, axis=0), in_=x1b.ap, in_offset=None,
                        bounds_check=BC(e), oob_is_err=False), [x1b.b, IDX.b], [BXE], eng="pool")
                F(*x1t)
                yield
                CK(11, sm.ap[:, 0:164], [sm, BXE])
                CK(100 + it * 10 + qs, sm.ap[:, 0:164], [sm, BXE])

        pending = [None]

        def pump(n=1):
            g_ = pending[0]
            if g_ is None:
                return
            ph = P.phase
            for _ in range(n):
                try:
                    next(g_)
                except StopIteration:
                    pending[0] = None
                    break
            P.phase = ph

        CK(1, vec.ap, [vec])
        for it in range(NT):
            t0 = it * 512
            P.phase = f"a1_t{it}"
            for s in range(4):
                xs = xs_t[s % 2]
                DMA(xs.ap, x_d[t0 + s * 128:t0 + (s + 1) * 128, :], [], [xs])
                for g4 in range(2):
                    ps = PS()
                    for j in range(4):
                        kc = g4 * 4 + j
                        TR(ps.ap[:, j * 128:(j + 1) * 128], xs.ap[:, kc * 128:(kc + 1) * 128], identF.ap, [xs, identF], [ps])
                    CP(xT3[:, g4 * 4:(g4 + 1) * 4, s * 128:(s + 1) * 128], ps.ap.rearrange("p (k t) -> p k t", k=4), [ps], [xT],
                       eng=("act" if g4 else "dve"))
            DMA(ropeC.ap, cd["ropeC"][:, t0:t0 + 512], [], [ropeC])
            DMA(ropeS.ap, cd["ropeS"][:, t0:t0 + 512], [], [ropeS])

            CK(2, xT.ap.bitcast(F32)[:, 0:512], [xT])
            P.phase = f"rwkv_t{it}"
            sh12, sh13 = proj_shift_multi([12, 13])
            CK(3, sh12.ap[:, 0:512], [sh12])
            th = G()
            ACT(th.ap[0:64, 0:512], sh12.ap[0:64, 0:512], AF.Tanh, [sh12], [th])
            sg = G()
            ACT(sg.ap[:, 0:512], sh13.ap[:, 0:512], AF.Sigmoid, [sh13], [sg])
            F(sh13)
            for pr in range(4):
                pcs = slice(pr * 128, (pr + 1) * 128)
                ps_d, ps_a, ps_g = PS(), PS(), PS()
                MM(ps_d.ap, dup.ap[0:64, pcs], th.ap[0:64, 0:512], [dup, th], [ps_d])
                MM(ps_a.ap, dup.ap[64:128, pcs], sh12.ap[64:128, 0:512], [dup, sh12], [ps_a])
                MM(ps_g.ap, gup.ap[:, pcs], sg.ap[:, 0:512], [gup, sg], [ps_g])
                sgw, aT, gT = G(), G(), G()
                ACT(sgw.ap[:, 0:512], ps_d.ap, AF.Sigmoid, [ps_d, vec], [sgw], bias=vec.ap[:, V_W0 + pr:V_W0 + pr + 1])
                ACT(aT.ap[:, 0:512], ps_a.ap, AF.Sigmoid, [ps_a, vec], [aT], bias=vec.ap[:, V_A0 + pr:V_A0 + pr + 1])
                CP(gT.ap[:, 0:512], ps_g.ap, [ps_g], [gT], eng="act")
                Ls = G()
                for c4 in range(4):
                    cs = slice(c4 * 128, (c4 + 1) * 128)
                    P.op("dve", lambda e, o=Ls.ap[:, cs], d1=sgw.ap[:, cs]: e.tensor_tensor_scan(o, onesF.ap, d1, 0.0, ALU.mult, ALU.add),
                         [onesF.b, sgw.b], [Ls.b])
                Lp = G()
                TT(Lp.ap[:, 0:512], Ls.ap[:, 0:512], sgw.ap[:, 0:512], ALU.subtract, [Ls, sgw], [Lp])
                gC, E1, E2, E3 = G(), G(), G(), G()
                ACT(gC.ap[:, 0:4], Ls.ap[:, 0:512].rearrange("p (c t) -> p c t", c=4)[:, :, 127], AF.Exp, [Ls], [gC], scale=-C0)
                ACT(E1.ap[:, 0:512], Ls.ap[:, 0:512], AF.Exp, [Ls], [E1], scale=-C0)
                ACT(E2.ap[:, 0:512], Ls.ap[:, 0:512], AF.Exp, [Ls], [E2], scale=C0)
                ACT(E3.ap[:, 0:512], Lp.ap[:, 0:512], AF.Exp, [Lp], [E3], scale=-C0)
                F(sgw, Ls, Lp)
                pump()
                rS, kS, vS = proj_shift_multi([pr, 4 + pr, 8 + pr])
                pump()
                kk, t1 = G(), G()
                TS(kk.ap[:, 0:512], kS.ap[:, 0:512], vec.ap[:, V_KK + pr:V_KK + pr + 1], None, ALU.mult, None, [kS, vec], [kk])
                TS(t1.ap[:, 0:512], aT.ap[:, 0:512], 1.0, vec.ap[:, V_KA + pr:V_KA + pr + 1], ALU.subtract, ALU.mult, [aT, vec], [t1])
                Rt = G()
                TT(b16(Rt), rS.ap[:, 0:512], E1.ap[:, 0:512], ALU.mult, [rS, E1], [Rt])
                sq = G()
                TT(sq.ap[:, 0:512], kk.ap[:, 0:512], kk.ap[:, 0:512], ALU.mult, [kk], [sq])
                kmod = G()
                STT(kmod.ap[:, 0:512], t1.ap[:, 0:512], 1.0, kS.ap[:, 0:512], ALU.add, ALU.mult, [t1, kS], [kmod])
                F(kS, E1)
                ps_n = PS()
                MM(ps_n.ap, bones.ap, sq.ap[:, 0:512], [bones, sq], [ps_n])
                STT(t1.ap[:, 0:512], rS.ap[:, 0:512], vec.ap[:, V_RK + pr:V_RK + pr + 1], kmod.ap[:, 0:512], ALU.mult, ALU.mult, [rS, vec, kmod], [t1])
                Kt = G()
                TT(b16(Kt), kmod.ap[:, 0:512], E2.ap[:, 0:512], ALU.mult, [kmod, E2], [Kt])
                TS(sq.ap[:, 0:512], ps_n.ap, 1e-24, None, ALU.max, None, [ps_n], [sq])
                ACT(sq.ap[:, 0:512], sq.ap[:, 0:512], AF.Ln, [sq], [sq])
                ps_b = PS()
                MM(ps_b.ap, bones.ap, t1.ap[:, 0:512], [bones, t1], [ps_b])
                F(rS, kmod)
                pump()
                ACT(sq.ap[:, 0:512], sq.ap[:, 0:512], AF.Exp, [sq], [sq], scale=-0.5)
                bon = G()
                TT(bon.ap[:, 0:512], ps_b.ap, vS.ap[:, 0:512], ALU.mult, [ps_b, vS], [bon])
                TT(kk.ap[:, 0:512], kk.ap[:, 0:512], sq.ap[:, 0:512], ALU.mult, [kk, sq], [kk])
                F(sq, t1)
                At = G()
                STT(b16(At), kk.ap[:, 0:512], -1.0, E3.ap[:, 0:512], ALU.mult, ALU.mult, [kk, E3], [At])
                bT = G()
                TT(bT.ap[:, 0:512], kk.ap[:, 0:512], aT.ap[:, 0:512], ALU.mult, [kk, aT], [bT])
                F(E3, aT, kk)
                Bt = G()
                TT(b16(Bt), bT.ap[:, 0:512], E2.ap[:, 0:512], ALU.mult, [bT, E2], [Bt])
                F(bT, E2)
                vSb, Hb = G(), G()
                CP(b16(vSb), vS.ap[:, 0:512], [vS], [vSb], eng="pool")
                CP(Hb.ap.bitcast(BF16)[:, 0:128], Hs[pr].ap, [Hs[pr]], [Hb], eng="pool")
                CK(4, At.ap[:, 0:512], [At])
                pump()
                gmk = G()
                for c4 in range(4):
                    TS(gmk.ap[:, c4 * 128:(c4 + 1) * 128], bones.ap, gC.ap[:, c4:c4 + 1], None, ALU.mult, None, [bones, gC], [gmk], eng="pool")
                psY = psb[7]
                H = Hs[pr]
                m_su_sl = mskA.ap.rearrange("p (a b c) -> p a b c", a=2, b=2)[:, :, 0, :]
                m_su_sui = mskB.ap.rearrange("p (a b c) -> p a b c", a=2, b=2)[:, :, 0, :]
                CSL = [slice(c4 * 128, (c4 + 1) * 128) for c4 in range(4)]
                st = [dict(CH[c4]) for c4 in range(4)]
                for c4 in range(4):
                    cs, c = CSL[c4], st[c4]
                    ps = PS()
                    psv = ps.ap.bitcast(BF16)
                    for j, src in enumerate((Kt, Bt, At, vSb)):
                        TR(psv[:, j * 128:(j + 1) * 128], b16(src)[:, cs], identB.ap, [src, identB], [ps])
                    CP(c["tok"].ap, psv[:, 0:512], [ps], [c["tok"]], eng="act")
                    c["tk"] = c["tok"].ap.rearrange("p (j c) -> p j c", j=4)
                for c4 in range(4):
                    cs, c = CSL[c4], st[c4]
                    NL, M2, M3 = c["NL"], c["M2"], c["M3"]
                    NLv = NL.ap.rearrange("p (a h c) -> p a h c", a=2, h=2)
                    M2v = M2.ap.rearrange("p (a h c) -> p a h c", a=2, h=2)
                    for hh in range(2):
                        hp = slice(64 * hh, 64 * hh + 64)
                        pa, pb = PS(), PS()
                        MM(pa.ap[:, 0:128], b16(Bt)[hp, cs], b16(At)[hp, cs], [Bt, At], [pa])
                        MM(pa.ap[:, 128:256], b16(At)[hp, cs], b16(Bt)[hp, cs], [Bt, At], [pa])
                        MM(pa.ap[:, 256:384], b16(Kt)[hp, cs], b16(At)[hp, cs], [Kt, At], [pa])
                        MM(pa.ap[:, 384:512], b16(Bt)[hp, cs], b16(Rt)[hp, cs], [Bt, Rt], [pa])
                        MM(pb.ap[:, 0:128], b16(Kt)[hp, cs], b16(Rt)[hp, cs], [Kt, Rt], [pb])
                        TT(NLv[:, :, hh, :], pa.ap[:, 0:256].rearrange("p (a c) -> p a c", a=2), m_su_sl, ALU.mult, [pa, mskA], [NL])
                        TT(M2v[:, :, hh, :], pa.ap[:, 256:512].rearrange("p (a c) -> p a c", a=2), m_su_sui, ALU.mult, [pa, mskB], [M2],
                           eng=("dve" if hh else "pool") if False else "dve")
                        TT(M3.ap[:, hh * 128:hh * 128 + 128], pb.ap[:, 0:128], mskB.ap[:, 256:384], ALU.mult, [pb, mskB], [M3])
                    pump()
                for c4 in range(4):
                    c = st[c4]
                    tk, M2, Wa = c["tk"], c["M2"], c["Wa"]
                    ps4 = PS()
                    for hh in range(2):
                        MM(ps4.ap[:, 64 + hh * 64:128 + hh * 64], M2.ap[:, hh * 128:hh * 128 + 128], tk[:, 3, hh * 64:hh * 64 + 64], [M2, c["tok"]], [ps4])
                    CP(Wa.ap[:, 0:64], tk[:, 2, 0:64], [c["tok"]], [Wa], eng="act")
                    CP(Wa.ap[:, 192:256], tk[:, 2, 64:128], [c["tok"]], [Wa], eng="act")
                    CP(Wa.ap[:, 64:192], ps4.ap[:, 64:192], [ps4], [Wa], eng="act")
                for j in range(7):
                    pump()
                    for c4 in range(4):
                        c = st[c4]
                        NL, Wa, Wb = c["NL"], c["Wa"], c["Wb"]
                        ps5 = PS()
                        for hh in range(2):
                            MM(ps5.ap[:, hh * 128:hh * 128 + 128], NL.ap[:, hh * 128:hh * 128 + 128], Wa.ap[:, hh * 128:hh * 128 + 128], [NL, Wa], [ps5])
                        TT(Wb.ap, ps5.ap[:, 0:256], Wa.ap, ALU.add, [ps5, Wa], [Wb])
                        c["Wa"], c["Wb"] = Wb, Wa
                    if j < 6:
                        for c4 in range(4):
                            c = st[c4]
                            NL, NL2 = c["NL"], c["NL2"]
                            ps6 = PS()
                            for hh in range(2):
                                o0, o1 = hh * 128, 256 + hh * 128
                                MM(ps6.ap[:, o0:o0 + 128], NL.ap[:, o1:o1 + 128], NL.ap[:, o0:o0 + 128], [NL], [ps6])
                                MM(ps6.ap[:, o1:o1 + 128], NL.ap[:, o0:o0 + 128], NL.ap[:, o1:o1 + 128], [NL], [ps6])
                            CP(NL2.ap, ps6.ap, [ps6], [NL2], eng="act")
                            c["NL"], c["NL2"] = NL2, NL
                for c4 in range(4):
                    c = st[c4]
                    Wa, AbT = c["Wa"], c["AbT"]
                    ps7 = PS()
                    p7v = ps7.ap.bitcast(BF16)
                    for hh in range(2):
                        TR(p7v[:, hh * 128:hh * 128 + 128], Wa.ap[:, hh * 128:hh * 128 + 128], identB.ap, [Wa, identB], [ps7])
                    CP(AbT.ap[0:64, 0:128], p7v[0:64, 0:128], [ps7], [AbT], eng="act")
                    CP(AbT.ap[64:128, 0:128], p7v[64:128, 128:256], [ps7], [AbT], eng="act")
                for c4 in range(4):
                    cs, c = CSL[c4], st[c4]
                    tk, M2, M3, Wa, AbT = c["tk"], c["M2"], c["M3"], c["Wa"], c["AbT"]
                    psU = PS()
                    Hbv = Hb.ap.bitcast(BF16)[:, 0:128]
                    MM(psU.ap[:, 0:128], AbT.ap[:, 0:128], Hbv, [AbT, Hb], [psU])
                    Ut = G()
                    U = Tl(Ut.ap.bitcast(BF16), Ut.b)
                    TT(U.ap[:, 0:128], psU.ap[:, 0:128], Wa.ap[:, 64:192], ALU.add, [psU, Wa], [U])
                    MM(psY.ap[:, cs], Hbv, b16(Rt)[:, cs], [Hb, Rt], [psY], start=True, stop=False)
                    for hh in range(2):
                        hp = slice(64 * hh, 64 * hh + 64)
                        yo = psY.ap[hp, cs]
                        MM(yo, U.ap[:, hh * 64:hh * 64 + 64], M2.ap[:, 256 + hh * 128:256 + hh * 128 + 128], [U, M2], [psY],
                           start=False, stop=False, tp=(0, 64 * hh))
                        MM(yo, tk[:, 3, hh * 64:hh * 64 + 64], M3.ap[:, hh * 128:hh * 128 + 128], [c["tok"], M3], [psY],
                           start=False, stop=True, tp=(0, 64 * hh))
                    psH = PS()
                    MM(psH.ap[:, 0:128], tk[:, 0, :], tk[:, 3, :], [c["tok"]], [psH], start=True, stop=False)
                    MM(psH.ap[:, 0:128], tk[:, 1, :], U.ap[:, 0:128], [c["tok"], U], [psH], start=False, stop=True)
                    tmpH = G()
                    TT(tmpH.ap[:, 0:128], psH.ap[:, 0:128], H.ap, ALU.add, [psH, H], [tmpH])
                    TT(Hbv, tmpH.ap[:, 0:128], gmk.ap[:, c4 * 128:(c4 + 1) * 128], ALU.mult, [tmpH, gmk], [Hb])
                    TT(H.ap, tmpH.ap[:, 0:128], gmk.ap[:, c4 * 128:(c4 + 1) * 128], ALU.mult, [tmpH, gmk], [H], eng="pool")
                    F(tmpH, Ut)
                    pump()
                F(Rt, Kt, Bt, At, gC, vSb, Hb, gmk)
                Yt = G()
                CP(Yt.ap[:, 0:512], psY.ap, [psY], [Yt], eng="act")
                ps = PS()
                MM(ps.ap, bo64.ap, Yt.ap[:, 0:512], [bo64, Yt], [ps])
                TT(Yt.ap[:, 0:512], Yt.ap[:, 0:512], ps.ap, ALU.subtract, [Yt, ps], [Yt])
                sq2 = G()
                TT(sq2.ap[:, 0:512], Yt.ap[:, 0:512], Yt.ap[:, 0:512], ALU.mult, [Yt], [sq2], eng="pool")
                ps = PS()
                MM(ps.ap, bo64.ap, sq2.ap[:, 0:512], [bo64, sq2], [ps])
                TS(sq2.ap[:, 0:512], ps.ap, GN_EPS, None, ALU.add, None, [ps], [sq2])
                ACT(sq2.ap[:, 0:512], sq2.ap[:, 0:512], AF.Ln, [sq2], [sq2])
                ACT(sq2.ap[:, 0:512], sq2.ap[:, 0:512], AF.Exp, [sq2], [sq2], scale=-0.5)
                TT(Yt.ap[:, 0:512], Yt.ap[:, 0:512], sq2.ap[:, 0:512], ALU.mult, [Yt, sq2], [Yt])
                TS(Yt.ap[:, 0:512], Yt.ap[:, 0:512], vec.ap[:, V_GG + pr:V_GG + pr + 1], vec.ap[:, V_GB + pr:V_GB + pr + 1], ALU.mult, ALU.add, [Yt, vec], [Yt])
                TT(Yt.ap[:, 0:512], Yt.ap[:, 0:512], bon.ap[:, 0:512], ALU.add, [Yt, bon], [Yt])
                TT(yrT3[:, pr, :], Yt.ap[:, 0:512], gT.ap[:, 0:512], ALU.mult, [Yt, gT], [yrT])
                F(Yt, sq2, bon, gT, vS)
            F(sh12, th, sg)
            CK(6, yrT.ap.bitcast(F32)[:, 0:512], [yrT])
            CK(60 + it, yrT.ap.bitcast(F32)[:, 0:512], [yrT])

            pump(10 ** 6)
            P.phase = f"attnprep_t{it}"
            P.barrier()
            MSET(vst.ap, 1.0, [vst])
            qr = [None] * 4
            for g2 in range(2):
                prs = (2 * g2, 2 * g2 + 1)
                keys = [(pr, which) for pr in prs for which in range(3)]
                pss, fs, rps, t2s, vps = {}, {}, {}, {}, {}
                for key in keys:
                    pss[key] = PS()
                    proj(14 + 4 * key[1] + key[0], pss[key])
                for key in keys:
                    fs[key] = G()
                    CP(fs[key].ap[:, 0:512], pss[key].ap, [pss[key]], [fs[key]], eng="act")
                for pr in prs:
                    for which in (0, 1):
                        rps[(pr, which)] = PS()
                        MM(rps[(pr, which)].ap, RmT.ap, fs[(pr, which)].ap[:, 0:512], [RmT, fs[(pr, which)]], [rps[(pr, which)]])
                for pr in prs:
                    vps[pr] = PS()
                    fv = fs[(pr, 2)]
                    for s in range(4):
                        TR(vps[pr].ap[:, s * 128:(s + 1) * 128], fv.ap[:, s * 128:(s + 1) * 128], identF.ap, [fv, identF], [vps[pr]])
                for key, ps in rps.items():
                    f = fs[key]
                    t2s[key] = G()
                    TT(t2s[key].ap[:, 0:512], ps.ap, ropeS.ap, ALU.mult, [ps, ropeS], [t2s[key]])
                    TT(f.ap[:, 0:512], f.ap[:, 0:512], ropeC.ap, ALU.mult, [f, ropeC], [f], eng="pool")
                for pr in prs:
                    CP(vst4[:, :, 2 * pr:2 * pr + 2, 0:64], vps[pr].ap.rearrange("p (s h d) -> p s h d", s=4, h=2), [vps[pr]], [vst])
                    F(fs[(pr, 2)])
                for key in rps:
                    f = fs[key]
                    TT(f.ap[:, 0:512], f.ap[:, 0:512], t2s[key].ap[:, 0:512], ALU.add, [f, t2s[key]], [f])
                    F(t2s[key])
                for pr in prs:
                    fq, fk = fs[(pr, 0)], fs[(pr, 1)]
                    CP(QT3[:, pr, :], fq.ap[:, 0:512], [fq], [QT], eng="act")
                    qr[pr] = fq
                    P.op("dve", lambda e, o=km[pr].ap[:, 2 * it:2 * it + 2], i_=fk.ap[:, 0:512].rearrange("p (b t) -> p b t", b=2):
                         e.tensor_reduce(o, i_, AX.X, ALU.add), [fk.b], [km[pr].b])
                    TS(km[pr].ap[:, 2 * it:2 * it + 2], km[pr].ap[:, 2 * it:2 * it + 2], 1.0 / 256.0, None, ALU.mult, None, [km[pr]], [km[pr]])
                    CP(kst.ap, fk.ap[:, 0:512], [fk], [kst], eng="act")
                    DMA(KT_d[pr][:, t0:t0 + 512], kst.ap, [kst], [BKT])
                    F(fk)
            DMA(V_d[4 * it:4 * it + 4].rearrange("s p c -> p s c"), vst.ap.rearrange("p (s c) -> p s c", s=4), [vst], [BV])
            stat = {}
            for ob in (2 * it, 2 * it + 1):
                t_ = G()
                TS(t_.ap[:, 0:128], jidx.ap, float(ob), -1e30, ALU.is_ge, ALU.mult, [jidx], [t_])
                TS(t_.ap[:, 128:256], jidx.ap, float(ob), None, ALU.is_lt, None, [jidx], [t_])
                TS(t_.ap[:, 256:384], jidx.ap, float(ob), None, ALU.is_equal, None, [jidx], [t_])
                stat[ob] = t_
            gms = [G() for _ in range(4)]
            for grp in ((0, 1), (2, 3)):
                psgs = {}
                for qs in grp:
                    psgs[qs] = [PS(), PS()]
                    for hh in range(2):
                        hp = slice(64 * hh, 64 * hh + 64)
                        for pr in range(4):
                            MM(psgs[qs][hh].ap[:, pr * 16:pr * 16 + 16], qr[pr].ap[hp, qs * 128:(qs + 1) * 128], km[pr].ap[hp, 0:16],
                               [qr[pr], km[pr]], [psgs[qs][hh]])
                for qs in grp:
                    ob = 2 * it + qs // 2
                    gmv = gms[qs].ap[:, 0:128].rearrange("p (r h j) -> p r h j", r=4, h=2)
                    ngv = stat[ob].ap[:, 0:128].rearrange("p (r h j) -> p r h j", r=4, h=2)
                    for hh in range(2):
                        TT(gmv[:, :, hh, :], ngv[:, :, hh, :], psgs[qs][hh].ap[:, 0:64].rearrange("p (r j) -> p r j", r=4), ALU.add,
                           [stat[ob], psgs[qs][hh]], [gms[qs]])
            for h in range(8):
                for qs in range(4):
                    gm3 = gms[qs].ap[:, 0:128].rearrange("p (h j) -> p h j", h=8)
                    m8 = gms[qs].ap[:, 128:192].rearrange("p (h j) -> p h j", h=8)
                    P.op("dve", lambda e, o=m8[:, h, :], i_=gm3[:, h, :]: e.max(out=o, in_=i_), [gms[qs].b], [gms[qs].b])
            for qs in range(4):
                gm3 = gms[qs].ap[:, 0:128].rearrange("p (h j) -> p h j", h=8)
                m8 = gms[qs].ap[:, 128:192].rearrange("p (h j) -> p h j", h=8)
                sel = gms[qs].ap[:, 192:320].rearrange("p (h j) -> p h j", h=8)
                TT(sel, gm3, m8[:, :, 2:3].to_broadcast([128, 8, 16]), ALU.is_ge, [gms[qs]], [gms[qs]])
            for qs in range(4):
                ob = 2 * it + qs // 2
                TT(gms[qs].ap[:, 192:320], gms[qs].ap[:, 192:320], stat[ob].ap[:, 128:256], ALU.mult, [gms[qs], stat[ob]], [gms[qs]])
            for qs in range(4):
                ob = 2 * it + qs // 2
                sel = gms[qs].ap[:, 192:320].rearrange("p (h j) -> p h j", h=8)
                TT(selm4[:, qs, :, :], sel, stat[ob].ap[:, 256:384].rearrange("p (h j) -> p h j", h=8), ALU.add, [gms[qs], stat[ob]], [selm])
            F(*gms, *stat.values())
            for f in qr:
                F(f)
            P.phase = f"attn_t{it}"
            nk = (it + 1) * 512
            pti_ = [0]

            def att_s1(pr, j):
                pts = {}
                pss = {}
                for u in range(2):
                    kt = 2 * j + u
                    for hh in range(2):
                        hp = slice(64 * hh, 64 * hh + 64)
                        pss[(hh, u)] = PS()
                        MM(pss[(hh, u)].ap, KTp.ap[hp, kt * 128:(kt + 1) * 128], QT3[hp, pr, :], [KTp, QT], [pss[(hh, u)]])
                for u in range(2):
                    kt = 2 * j + u
                    kl = kt - 4 * it
                    for hh in range(2):
                        pt = PTb[pti_[0] % 8]
                        pti_[0] += 1
                        ACT(pt.ap, pss[(hh, u)].ap, AF.Exp, [pss[(hh, u)]], [pt], scale=0.125)
                        if kl >= 0:
                            TT(pt.ap, pt.ap, cmask.ap[:, kl * 512:(kl + 1) * 512], ALU.mult, [pt, cmask], [pt])
                        pts[(hh, u)] = pt
                return pts

            def att_s2(pr, j, pts):
                q0 = 2 if j == 2 * it + 1 else 0
                nq = 4 - q0
                for hh in range(2):
                    h = 2 * pr + hh
                    psO = PS()
                    for qs in range(q0, 4):
                        for u in range(2):
                            kt = 2 * j + u
                            MM(psO.ap[:, qs * 128:qs * 128 + 65], pts[(hh, u)].ap[:, qs * 128:(qs + 1) * 128], Vp4[:, kt, hh, 0:65],
                               [pts[(hh, u)], Vp], [psO], start=(u == 0), stop=(u == 1))
                    tmp = G()
                    tv = tmp.ap[:, 0:nq * 65].rearrange("p (q d) -> p q d", q=nq)
                    TT(tv, psO.ap.rearrange("p (q d) -> p q d", q=4)[:, q0:4, 0:65],
                       selm4[:, q0:4, h, j:j + 1].to_broadcast([128, nq, 65]), ALU.mult, [psO, selm], [tmp])
                    TT(acc4[:, hh, q0:4, 0:65], acc4[:, hh, q0:4, 0:65], tv, ALU.add, [acc, tmp], [acc])
                    F(tmp)

            for pr in range(4):
                DMA(KTp.ap[:, 0:nk], KT_d[pr][:, 0:nk], [BKT], [KTp])
                DMA(Vp4[:, 0:4 * (it + 1), :, :],
                    V_d[0:4 * (it + 1)].rearrange("s p (h d) -> p s h d", h=8)[:, :, 2 * pr:2 * pr + 2, :], [BV], [Vp])
                MSET(acc.ap, 0.0, [acc])
                nj = 2 * it + 2
                nxt = att_s1(pr, 0)
                for j in range(nj):
                    cur = nxt
                    if j + 1 < nj:
                        nxt = att_s1(pr, j + 1)
                    att_s2(pr, j, cur)
                rden = G()
                rd3 = rden.ap[:, 0:8].rearrange("p (h q) -> p h q", h=2)
                RCP(rd3, acc4[:, :, :, 64], [acc], [rden])
                atq = G()
                atv = atq.ap[:, 0:512].rearrange("p (q h d) -> p q h d", q=4, h=2)
                for qs in range(4):
                    TT(atv[:, qs, :, :], acc4[:, :, qs, 0:64], rd3[:, :, qs:qs + 1].to_broadcast([128, 2, 64]), ALU.mult, [acc, rden], [atq])
                ps = PS()
                for qs in range(4):
                    TR(ps.ap[:, qs * 128:(qs + 1) * 128], atq.ap[:, qs * 128:(qs + 1) * 128], identF.ap, [atq, identF], [ps])
                CP(aoT3[:, pr, :], ps.ap, [ps], [aoT], eng="act")
                F(rden, atq)

            CK(7, aoT.ap.bitcast(F32)[:, 0:512], [aoT])
            CK(70 + it, aoT.ap.bitcast(F32)[:, 0:512], [aoT])
            P.barrier()
            P.phase = f"merge_t{it}"
            for cc in range(8):
                ccs = slice(cc * 128, (cc + 1) * 128)
                psr, psa = PS(), PS()
                for pr in range(4):
                    MM(psr.ap, Wbr3[:, pr, ccs], yrT3[:, pr, :], [Wbr, yrT], [psr], start=(pr == 0), stop=(pr == 3))
                for pr in range(4):
                    MM(psa.ap, Wba3[:, pr, ccs], aoT3[:, pr, :], [Wba, aoT], [psa], start=(pr == 0), stop=(pr == 3))
                ps = PS()
                proj(26 + cc, ps)
                g0 = G()
                ACT(g0.ap[:, 0:512], ps.ap, AF.Sigmoid, [ps], [g0])
                ps = PS()
                proj(34 + cc, ps)
                g1 = G()
                ACT(g1.ap[:, 0:512], ps.ap, AF.Sigmoid, [ps], [g1])
                TT(g0.ap[:, 0:512], g0.ap[:, 0:512], psr.ap, ALU.mult, [g0, psr], [g0])
                TT(g1.ap[:, 0:512], g1.ap[:, 0:512], psa.ap, ALU.mult, [g1, psa], [g1])
                TT(mg3[:, cc, :], g0.ap[:, 0:512], g1.ap[:, 0:512], ALU.add, [g0, g1], [mg], eng="pool")
                F(g0, g1)
            CK(8, mg.ap.bitcast(F32)[:, 0:512], [mg])
            pending[0] = ln_route(it, t0)
            if it == NT - 1 or stop != 0:
                pump(10 ** 6)

        if stage == 1:
            P.barrier()
            for s in range(NS):
                t = xs_t[s % 2]
                DMA(t.ap, X1_d[s * 128:(s + 1) * 128, :], [BX1], [t])
                DMA(out_d[s * 128:(s + 1) * 128, :], t.ap, [t], [Bout])
            P.barrier()
            P.emit()
            return nc

        P.barrier()
        A.off = mark_all
        WinB = [A.bf(8 * 2048, f"Win{i}") for i in range(2)]
        WoutB = [A.bf(8 * D, f"Wout{i}") for i in range(2)]
        binB = A.f32(32 * 16, "bin")
        DMA(binB.ap, ebin_d, [], [binB])
        bin3 = binB.ap.rearrange("p (e f) -> p e f", e=32)
        boutB = [A.f32(D, f"bout{i}") for i in range(2)]
        XK = [A.bf(NBLK * D, f"xe_tok{i}") for i in range(2)]
        xeT = A.bf(8 * CAP, "xeT")
        xeT3 = xeT.ap.rearrange("p (k s) -> p k s", k=8)
        actT = A.bf(8 * CAP, "actT")
        actT3 = actT.ap.rearrange("p (k s) -> p k s", k=8)
        eg = [A.f32(HALF, f"eg{i}") for i in range(2)]
        es = [A.f32(HALF, f"es{i}") for i in range(2)]
        el = [A.f32(HALF, f"el{i}") for i in range(2)]
        yo = [A.f32(D, f"yo{i}") for i in range(2)]
        print("stage B arena words", A.off)
        psbB = [Tl(t.ap.bitcast(BF16), t.b) for t in psb]
        SR = [A.f32(2048, f"stg{i}") for i in range(3)]
        sri = [0]

        def make_loader(e_):
            Win, Wout, bout = WinB[e_ % 2], WoutB[e_ % 2], boutB[e_ % 2]
            Win3 = Win.ap.rearrange("p (k f) -> p k f", k=8)
            Wout3 = Wout.ap.rearrange("p (k c) -> p k c", k=8)
            pieces = []
            for kc in range(8):
                pieces.append((Win3[:, kc, :], ewin_d[e_][kc * 128:(kc + 1) * 128, :], Win, None))
            wo_src = ewout_d[e_].rearrange("(k p) c -> p k c", p=128)
            for j in range(4):
                pieces.append((Wout3[:, 2 * j:2 * j + 2, :], wo_src[:, 2 * j:2 * j + 2, :], Wout, 2))
            stg = []

            def issue(i):
                dst, src, tl, k2 = pieces[i]
                t = SR[sri[0] % 3]
                sri[0] += 1
                v = t.ap if k2 is None else t.ap.rearrange("p (k c) -> p k c", k=2)
                DMA(v, src, [], [t], eng="pool")
                stg.append((t, v))

            def start():
                DMA(bout.ap, ebout_d[e_:e_ + 1, :].partition_broadcast(128), [], [bout])
                issue(0)
                issue(1)

            def step(i):
                if i >= len(pieces):
                    return
                dst, src, tl, k2 = pieces[i]
                t, v = stg[i]
                CP(dst, v, [t], [tl], eng="act")
                if i + 2 < len(pieces):
                    issue(i + 2)

            return start, step

        def xe_load(e_):
            xk = XK[e_ % 2]
            DMA(xk.ap.rearrange("p (b c) -> p b c", b=NBLK), XE_d[e_ * CAP:(e_ + 1) * CAP, :].rearrange("(b p) c -> p b c", p=128), [BXE], [xk])

        def xe_transpose(e_):
            xk = XK[e_ % 2]
            xk3 = xk.ap.rearrange("p (b c) -> p b c", b=NBLK)
            for b_ in range(NBLK):
                for g4 in range(2):
                    ps = psb[psi[0] % 8]
                    psB = psbB[psi[0] % 8]
                    psi[0] += 1
                    for j in range(4):
                        kc = g4 * 4 + j
                        TR(psB.ap[:, j * 128:(j + 1) * 128], xk3[:, b_, kc * 128:(kc + 1) * 128], identB.ap, [xk, identB], [ps])
                    CP(xeT3[:, g4 * 4:(g4 + 1) * 4, b_ * 128:(b_ + 1) * 128], psB.ap[:, 0:512].rearrange("p (k t) -> p k t", k=4), [ps], [xeT],
                       eng=("act" if g4 else "dve"))

        xe_load(0)
        xe_transpose(0)
        P.phase = "expert0"
        ld_start, ld_step = make_loader(0)
        ld_start()
        for i in range(12):
            ld_step(i)
        for e_ in range(32):
            P.phase = f"expert{e_}"
            if e_ + 1 < 32:
                ld_start, ld_step = make_loader(e_ + 1)
                ld_start()
            else:
                ld_step = lambda i: None
            unit = [0]
            Win, Wout, bout = WinB[e_ % 2], WoutB[e_ % 2], boutB[e_ % 2]
            Win3 = Win.ap.rearrange("p (k f) -> p k f", k=8)
            Wout3 = Wout.ap.rearrange("p (k c) -> p k c", k=8)
            if e_ + 1 < 32:
                xe_load(e_ + 1)
            for fc in range(8):
                for hf in range(2):
                    ss = slice(hf * HALF, (hf + 1) * HALF)
                    pg, pl = PS(), PS()
                    for kc in range(8):
                        MM(pg.ap[:, 0:HALF], Win3[:, kc, fc * 128:(fc + 1) * 128], xeT3[:, kc, ss], [Win, xeT], [pg], start=(kc == 0), stop=(kc == 7))
                    for kc in range(8):
                        MM(pl.ap[:, 0:HALF], Win3[:, kc, 1024 + fc * 128:1024 + (fc + 1) * 128], xeT3[:, kc, ss], [Win, xeT], [pl], start=(kc == 0), stop=(kc == 7))
                    g_, s_, l_ = eg[hf], es[hf], el[hf]
                    TS(g_.ap, pg.ap[:, 0:HALF], bin3[:, e_, fc:fc + 1], 7.0, ALU.add, ALU.min, [pg, binB], [g_])
                    ACT(s_.ap, g_.ap, AF.Sigmoid, [g_], [s_], scale=1.702)
                    TS(l_.ap, pl.ap[:, 0:HALF], bin3[:, e_, 8 + fc:9 + fc], 7.0, ALU.add, ALU.min, [pl, binB], [l_])
                    TS(l_.ap, l_.ap, -7.0, 1.0, ALU.max, ALU.add, [l_], [l_])
                    TT(g_.ap, g_.ap, s_.ap, ALU.mult, [g_, s_], [g_])
                    TT(actT3[:, fc, ss], g_.ap, l_.ap, ALU.mult, [g_, l_], [actT])
                    ld_step(unit[0])
                    unit[0] += 1
            if e_ + 1 < 32:
                xe_transpose(e_ + 1)
            for b_ in range(NBLK):
                y_ = yo[b_ % 2]
                for half in range(2):
                    ps = PS()
                    hs = slice(half * 512, (half + 1) * 512)
                    for fc in range(8):
                        MM(ps.ap, actT3[:, fc, b_ * 128:(b_ + 1) * 128], Wout3[:, fc, hs], [actT, Wout], [ps], start=(fc == 0), stop=(fc == 7))
                    TT(y_.ap[:, hs], ps.ap, bout.ap[:, hs], ALU.add, [ps, bout], [y_])
                DMA(Y_d[e_ * CAP + b_ * 128:e_ * CAP + (b_ + 1) * 128, :], y_.ap, [y_], [BY])

        P.barrier()
        P.phase = "combine"
        A.off = mark_all
        yk = [A.f32(D, f"yk{i}") for i in range(8)]
        x1c = [A.f32(D, f"x1c{i}") for i in range(2)]
        oc = [A.f32(D, f"oc{i}") for i in range(2)]
        sm2 = A.f32(64, "sm2")
        lnp2 = [A.f32(D, f"lnq{i}") for i in range(2)]
        for i in range(2):
            DMA(lnp2[i].ap, lnp_d[2 + i], [], [lnp2[i]])
        print("stage C arena words", A.off)

        def c_load(s):
            tsl = slice(s * 128, (s + 1) * 128)
            DMA(x1c[s % 2].ap, X1_d[tsl, :], [BX1], [x1c[s % 2]])
            for k in range(4):
                y_ = yk[(s % 2) * 4 + k]

                def gath(e, o=y_.ap, ix=IDX3[:, s, k:k + 1]):
                    return e.indirect_dma_start(out=o, out_offset=None, in_=Y_d, in_offset=bass.IndirectOffsetOnAxis(ap=ix, axis=0),
                                                bounds_check=BC(e), oob_is_err=False)
                P.dma(gath, [BY, IDX.b], [y_.b], eng="pool")

        c_load(0)
        for s in range(NS):
            tsl = slice(s * 128, (s + 1) * 128)
            if s + 1 < NS:
                c_load(s + 1)
            xc, o_ = x1c[s % 2], oc[s % 2]
            TS(xc.ap, xc.ap, ALPHA, None, ALU.mult, None, [xc], [xc])
            for k in range(4):
                y_ = yk[(s % 2) * 4 + k]
                STT(xc.ap, y_.ap, Wk3[:, s, k:k + 1], xc.ap, ALU.mult, ALU.add, [y_, Wk, xc], [xc])
            xh = [Tl(xc.ap[:, 0:512], xc.b), Tl(xc.ap[:, 512:1024], xc.b)]
            oh = [Tl(o_.ap[:, 0:512], o_.b), Tl(o_.ap[:, 512:1024], o_.b)]
            layer_norm(P, TS, TT, ACT, RCP, xh, oh, lnp2[0], lnp2[1], sm2)
            DMA(out_d[tsl, :], o_.ap, [o_], [Bout])
        P.barrier()
        P.emit()
    return nc


def layer_norm(P, TS, TT, ACT, RCP, src, dst, g, b, sm):
    st6 = sm.ap[:, 0:12].rearrange("p (c s) -> p c s", c=2)
    for c in range(2):
        P.op("dve", lambda e, o=st6[:, c, :], i_=src[c].ap[:, 0:512]: e.bn_stats(o, i_), [src[c].b], [sm.b])
    mv = sm.ap[:, 12:14]
    P.op("dve", lambda e: e.bn_aggr(mv, sm.ap[:, 0:12]), [sm.b], [sm.b])
    sd = sm.ap[:, 14:15]
    TS(sd, mv[:, 1:2], LN_EPS, None, ALU.add, None, [sm], [sm])
    ACT(sd, sd, AF.Ln, [sm], [sm])
    ACT(sd, sd, AF.Exp, [sm], [sm], scale=-0.5)
    for c in range(2):
        cs = slice(c * 512, (c + 1) * 512)
        TS(dst[c].ap[:, 0:512], src[c].ap[:, 0:512], mv[:, 0:1], sd, ALU.subtract, ALU.mult, [src[c], sm], [dst[c]])
        TT(dst[c].ap[:, 0:512], dst[c].ap[:, 0:512], g.ap[:, cs], ALU.mult, [dst[c], g], [dst[c]], eng="pool")
        TT(dst[c].ap[:, 0:512], dst[c].ap[:, 0:512], b.ap[:, cs], ALU.add, [dst[c], b], [dst[c]])


def prep_weights(inp):
    f = lambda a: np.ascontiguousarray(np.asarray(a, dtype=np.float32))
    w = {}
    w["w_in"] = f(inp["w_in"][0])
    cols = [f(inp["shift_mix"][0]).reshape(14, 128).T]
    for k in ("decay_w0", "iclr_a0", "k_k", "k_a", "r_k", "gn_g", "gn_b"):
        cols.append(f(inp[k][0]).reshape(4, 128).T)
    w["vec"] = f(np.concatenate(cols, 1))
    w["dup"] = f(np.concatenate([inp["decay_up"][0], inp["iclr_up"][0]], 0))
    w["gate_up"] = f(inp["gate_up"][0])
    w["w_branch_rwkv"] = f(inp["w_branch_rwkv"][0])
    w["w_branch_attn"] = f(inp["w_branch_attn"][0])
    w["w_out"] = f(inp["w_out"][0])
    lnp = np.stack([inp["ln1_g"][0], inp["ln1_b"][0], inp["ln2_g"][0], inp["ln2_b"][0]], 0)
    w["lnp"] = f(np.broadcast_to(np.asarray(lnp)[:, None, :], (4, 128, D)))
    w["router_w"] = f(inp["router_w"][0])
    w["router_b_bc"] = f(np.broadcast_to(np.asarray(inp["router_b"][0])[None, :], (128, 32)))
    w["expert_w_in"] = f(inp["expert_w_in"][0])
    bi = f(inp["expert_b_in"][0]).reshape(32, 16, 128)
    w["expert_b_in_fm"] = f(bi.transpose(2, 0, 1).reshape(128, 32 * 16))
    w["expert_w_out"] = f(inp["expert_w_out"][0])
    w["expert_b_out"] = f(inp["expert_b_out"][0])
    return w


_NC_CACHE = {}


def kernel(**inputs):
    x = np.asarray(inputs["x"], dtype=np.float32)
    B, T, _ = x.shape
    w = prep_weights(inputs)
    for k, v in host_consts(T).items():
        w["c_" + k] = np.ascontiguousarray(v, dtype=np.float32)
    if T not in _NC_CACHE:
        _NC_CACHE[T] = build(T)
    nc = _NC_CACHE[T]
    in_maps = []
    for b in range(B):
        m = dict(w)
        m["x"] = np.ascontiguousarray(x[b])
        in_maps.append(m)
    res = run_bass_kernel_spmd(nc, in_maps, core_ids=list(range(B)))
    return np.stack([np.asarray(r["out"], dtype=np.float32) for r in res.results], 0)
```

```python
from contextlib import ExitStack
import numpy as np
import concourse.bass as bass
import concourse.mybir as mybir
from concourse.bass_utils import run_bass_kernel_spmd

F32 = mybir.dt.float32
BF16 = mybir.dt.bfloat16
I32 = mybir.dt.int32
ALU = mybir.AluOpType
AF = mybir.ActivationFunctionType
AX = mybir.AxisListType

D = 1024
ALPHA = 2.0 ** 0.25
C0 = float(np.exp(-0.5))
GN_EPS = 64e-5
LN_EPS = 1e-5
ENGS = ("pe", "act", "dve", "pool", "sp")


class Buf:
    __slots__ = ("name", "lw", "rd")

    def __init__(self, name):
        self.name = name
        self.lw = None
        self.rd = []


class Tl:
    __slots__ = ("ap", "b")

    def __init__(self, ap, b):
        self.ap = ap
        self.b = b


class Prog:
    NSLOT = 8

    def __init__(self, nc):
        self.nc = nc
        self.q = {e: [] for e in ENGS}
        self.cnt = {e: 0 for e in ENGS}
        self.seen = {e: {} for e in ENGS}
        self.dma_i = {"sp": 0, "pool": 0, "act": 0}
        self.dma_uses = {}
        self.nbuf = 0
        self.phase = None
        self.scopes = False

    def buf(self, name=None):
        self.nbuf += 1
        return Buf(name or f"b{self.nbuf}")

    def _waits(self, eng, reads, writes):
        w = {}

        def add(ev):
            if ev is None:
                return
            k, v = ev
            if eng == "pe" and k == "pe":
                return
            if w.get(k, 0) < v:
                w[k] = v

        for b in reads:
            add(b.lw)
        for b in writes:
            add(b.lw)
            for ev in b.rd:
                add(ev)
        return w

    def _commit(self, eng, w, reads, writes, ev):
        seen = self.seen[eng]
        out = []
        for k, v in w.items():
            if seen.get(k, 0) < v:
                seen[k] = v
                out.append((k, v))
        for b in reads:
            b.rd.append(ev)
            if len(b.rd) > 64:
                best = {}
                for k, v in b.rd:
                    if best.get(k, 0) < v:
                        best[k] = v
                b.rd = list(best.items())
        for b in writes:
            b.lw = ev
            b.rd = []
        return out

    def op(self, eng, fn, reads=(), writes=()):
        w = self._waits(eng, reads, writes)
        self.cnt[eng] += 1
        ev = (eng, self.cnt[eng])
        waits = self._commit(eng, w, reads, writes, ev)
        self.q[eng].append((waits, fn, (eng, 1), self.phase))
        return ev

    def dma(self, fn, reads=(), writes=(), eng="sp"):
        w = self._waits(eng, reads, writes)
        i = self.dma_i[eng]
        self.dma_i[eng] += 1
        slot = f"d_{eng}_{i % self.NSLOT}"
        uses = self.dma_uses.get(slot, 0)
        if uses > 0 and w.get(slot, 0) < 16 * uses:
            w[slot] = 16 * uses
        self.dma_uses[slot] = uses + 1
        ev = (slot, 16 * (uses + 1))
        waits = self._commit(eng, w, reads, writes, ev)
        self.q[eng].append((waits, fn, (slot, 16), self.phase))
        return ev

    def barrier(self):
        evs = [(e, self.cnt[e]) for e in ENGS if self.cnt[e] > 0]
        evs += [(s, 16 * u) for s, u in self.dma_uses.items()]
        for e in ENGS:
            waits = []
            for k, v in evs:
                if e == "pe" and k == "pe":
                    continue
                if self.seen[e].get(k, 0) < v:
                    self.seen[e][k] = v
                    waits.append((k, v))
            self.q[e].append((waits, None, None, None))

    def emit(self):
        nc = self.nc
        names = set()
        for e in ENGS:
            for waits, fn, inc, _ph in self.q[e]:
                for k, _ in waits:
                    names.add(k)
                if inc is not None:
                    names.add(inc[0])
        with ExitStack() as st:
            sems = {n: st.enter_context(nc.semaphore("s_" + n)) for n in sorted(names)}
            block = st.enter_context(nc.Block())
            q = self.q

            def run(eng_obj, ops):
                for waits, fn, inc, ph in ops:
                    for k, v in waits:
                        eng_obj.wait_ge(sems[k], v)
                    if fn is not None:
                        if self.scopes and ph is not None:
                            with nc.named_scope(ph):
                                fn(eng_obj).then_inc(sems[inc[0]], inc[1])
                        else:
                            fn(eng_obj).then_inc(sems[inc[0]], inc[1])

            @block.tensor
            def _(e):
                run(e, q["pe"])

            @block.scalar
            def _(e):
                run(e, q["act"])

            @block.vector
            def _(e):
                run(e, q["dve"])

            @block.gpsimd
            def _(e):
                run(e, q["pool"])

            @block.sync
            def _(e):
                run(e, q["sp"])


class Arena:
    def __init__(self, ap, P, words):
        self.ap, self.P, self.off, self.words = ap, P, 0, words

    def f32(self, n, name=None):
        n2 = (n + 1) // 2 * 2
        assert self.off + n2 <= self.words, f"arena overflow {name} {self.off}+{n2}>{self.words}"
        v = self.ap[:, self.off:self.off + n]
        self.off += n2
        return Tl(v, self.P.buf(name))

    def bf(self, n, name=None):
        w = (n + 1) // 2
        t = self.f32(w, name)
        return Tl(t.ap.bitcast(BF16)[:, 0:n], t.b)

    def i32(self, n, name=None):
        t = self.f32(n, name)
        return Tl(t.ap.bitcast(I32), t.b)


def cap_for(T):
    if T == 4096:
        return 640
    return max(256, ((T // 8) * 3 // 2 + 127) // 128 * 128)


def host_consts(T):
    c = {}
    c["identF"] = np.eye(128, dtype=np.float32)
    bo = np.zeros((128, 128), np.float32)
    bo[:64, :64] = 1.0
    bo[64:, 64:] = 1.0
    c["bones"] = bo
    p = np.arange(128)[:, None]
    q = np.arange(128)[None, :]
    su = (p < q).astype(np.float32)
    sl = (q < p).astype(np.float32)
    sui = (p <= q).astype(np.float32)
    c["mskA"] = np.concatenate([su, su, sl, sl], 1)
    c["mskB"] = np.concatenate([su, su, sui, sui], 1)
    c["striu"] = su.copy()
    half = 8
    inv = np.power(500000.0, -np.arange(0, 16, 2, dtype=np.float32) / 16.0).astype(np.float32)
    ang = np.arange(T, dtype=np.float32)[None, :] * inv[:, None]
    cosv, sinv = np.cos(ang).astype(np.float32), np.sin(ang).astype(np.float32)
    Ct = np.ones((128, T), np.float32)
    St = np.zeros((128, T), np.float32)
    Rm = np.zeros((128, 128), np.float32)
    for h in range(2):
        b0 = 64 * h
        Ct[b0:b0 + 8] = cosv
        Ct[b0 + 8:b0 + 16] = cosv
        St[b0:b0 + 8] = sinv
        St[b0 + 8:b0 + 16] = sinv
        for i in range(8):
            Rm[b0 + i, b0 + i + 8] = -1.0
            Rm[b0 + i + 8, b0 + i] = 1.0
    c["ropeC"], c["ropeS"] = Ct, St
    c["RmT"] = np.ascontiguousarray(Rm.T)
    cm = np.zeros((4, 128, 512), np.float32)
    tri = (p <= q).astype(np.float32)
    for kl in range(4):
        for qs in range(4):
            blk_k, blk_q = kl // 2, qs // 2
            if blk_k < blk_q:
                m = 1.0
            elif blk_k > blk_q:
                m = 0.0
            elif kl < qs:
                m = 1.0
            elif kl > qs:
                m = 0.0
            else:
                m = tri
            cm[kl, :, qs * 128:(qs + 1) * 128] = m
    c["cmask"] = cm.transpose(1, 0, 2).reshape(128, 2048).copy()
    c["ecap"] = np.tile((np.arange(32, dtype=np.float32) * cap_for(T))[None, :], (128, 1))
    return c


class _Done(Exception):
    pass


def build(T, stage=2, stop=0, scopes=False):
    try:
        return _build(T, stage, stop, scopes)
    except _Done as d:
        return d.args[0]


def _build(T, stage=2, stop=0, scopes=False):
    NT, NS = T // 512, T // 128
    CAP = cap_for(T)
    NBLK = CAP // 128
    HALF = CAP // 2
    NROW = 32 * CAP
    nc = bass.Bass("TRN2", target_bir_lowering=False)

    def din(name, shape, dt=F32):
        return nc.dram_tensor(name, list(shape), dt, kind="ExternalInput").ap()

    x_d = din("x", [T, D])
    w_in_d = din("w_in", [D, 5376])
    vec_d = din("vec", [128, 42])
    dup_d = din("dup", [128, 512])
    gup_d = din("gate_up", [128, 512])
    wbr_d = din("w_branch_rwkv", [512, D])
    wba_d = din("w_branch_attn", [512, D])
    wo_d = din("w_out", [D, D])
    lnp_d = din("lnp", [4, 128, D])
    rw_d = din("router_w", [D, 32])
    rb_d = din("router_b_bc", [128, 32])
    ewin_d = din("expert_w_in", [32, D, 2048])
    ebin_d = din("expert_b_in_fm", [128, 32 * 16])
    ewout_d = din("expert_w_out", [32, D, D])
    ebout_d = din("expert_b_out", [32, D])
    cst = host_consts(T)
    cd = {k: din("c_" + k, v.shape) for k, v in cst.items()}
    out_d = nc.dram_tensor("out", [T, D], F32, kind="ExternalOutput").ap()
    X1_d = nc.dram_tensor("X1s", [T, D], F32, kind="Internal").ap()
    XE_d = nc.dram_tensor("XEs", [NROW + 1, D], BF16, kind="Internal").ap()
    Y_d = nc.dram_tensor("Ys", [NROW + 1, D], F32, kind="Internal").ap()
    KT_d = nc.dram_tensor("KTs", [4, 128, T], BF16, kind="Internal").ap()
    Wb_d = nc.dram_tensor("Wbs", [42, 128, 1024], BF16, kind="Internal").ap()
    V_d = nc.dram_tensor("Vs", [NS, 128, 8 * 66], BF16, kind="Internal").ap()

    P = Prog(nc)
    P.scopes = scopes
    WORDS = 51200
    with ExitStack() as st:
        arena_t = st.enter_context(nc.sbuf_tensor("arena", [128, WORDS], F32))
        A = Arena(arena_t[:, :], P, WORDS)
        psb = [Tl(st.enter_context(nc.psum_tensor(f"ps{i}", [128, 512], F32))[:, :], P.buf(f"ps{i}")) for i in range(8)]
        psi = [0]

        def PS():
            t = psb[psi[0] % 7]
            psi[0] += 1
            return t

        Bout = P.buf("out")
        BX1, BXE, BY = P.buf("X1"), P.buf("XE"), P.buf("Y")
        BKT, BV = P.buf("KTd"), P.buf("Vd")
        BWB = [P.buf(f"Wb{c}") for c in range(42)]

        def bl(ts):
            return [t.b if isinstance(t, Tl) else t for t in ts]

        def MM(out, lhsT, rhs, R, W, start=True, stop=True, tp=None):
            kw = {} if tp is None else {"tile_position": tp}
            P.op("pe", lambda e: e.matmul(out, lhsT, rhs, start=start, stop=stop, **kw), bl(R), bl(W))

        def TR(out, in_, ident, R, W):
            P.op("pe", lambda e: e.transpose(out, in_, ident), bl(R), bl(W))

        def ACT(out, in_, func, R, W, bias=None, scale=None, accum=None):
            kw = {}
            if bias is not None:
                kw["bias"] = bias
            if scale is not None:
                kw["scale"] = scale
            if accum is not None:
                kw["accum_out"] = accum
            P.op("act", lambda e: e.activation(out=out, in_=in_, func=func, **kw), bl(R), bl(W))

        def TT(out, a, b, op, R, W, eng="dve"):
            P.op(eng, lambda e: e.tensor_tensor(out=out, in0=a, in1=b, op=op), bl(R), bl(W))

        def TS(out, a, s1, s2, op0, op1, R, W, eng="dve", accum=None):
            if op1 is None:
                P.op(eng, lambda e: e.tensor_scalar(out, a, s1, None, op0), bl(R), bl(W))
            elif accum is None:
                P.op(eng, lambda e: e.tensor_scalar(out, a, s1, s2, op0, op1), bl(R), bl(W))
            else:
                P.op(eng, lambda e: e.tensor_scalar(out, a, s1, s2, op0, op1, accum), bl(R), bl(W))

        def STT(out, in0, scalar, in1, op0, op1, R, W, accum=None):
            if accum is None:
                P.op("dve", lambda e: e.scalar_tensor_tensor(out=out, in0=in0, scalar=scalar, in1=in1, op0=op0, op1=op1), bl(R), bl(W))
            else:
                P.op("dve", lambda e: e.scalar_tensor_tensor(out=out, in0=in0, scalar=scalar, in1=in1, op0=op0, op1=op1, accum_out=accum), bl(R), bl(W))

        def CP(out, in_, R, W, eng="dve"):
            if eng == "act":
                P.op("act", lambda e: e.activation(out=out, in_=in_, func=AF.Copy), bl(R), bl(W))
            else:
                P.op(eng, lambda e: e.tensor_copy(out, in_), bl(R), bl(W))

        def RCP(out, in_, R, W):
            P.op("dve", lambda e: e.reciprocal(out, in_), bl(R), bl(W))

        def MSET(ap, val, W, eng="pool"):
            P.op(eng, lambda e: e.memset(ap, val), [], bl(W))

        _bc = {}

        def BC(e):
            if "r" not in _bc:
                _bc["r"] = e.to_reg(NROW)
            return _bc["r"]

        def DMA(out, in_, R, W, eng="sp"):
            P.dma(lambda e: e.dma_start(out=out, in_=in_), bl(R), bl(W), eng=eng)

        def CK(k, ap=None, R=()):
            if stop != k:
                return
            if ap is not None:
                n = ap.shape[-1]
                DMA(out_d[0:ap.shape[0], 0:n], ap, list(R), [Bout])
            P.barrier()
            P.emit()
            raise _Done(nc)

        identF = A.f32(128, "identF")
        DMA(identF.ap, cd["identF"], [], [identF])
        identB = A.bf(128, "identB")
        CP(identB.ap, identF.ap, [identF], [identB])
        onesF = A.f32(128, "onesF")
        MSET(onesF.ap, 1.0, [onesF])
        lnp = [A.f32(D, f"lnp{i}") for i in range(2)]
        for i in range(2):
            DMA(lnp[i].ap, lnp_d[i], [], [lnp[i]])
        Wk = A.f32(NS * 4, "Wk")
        IDX = A.i32(NS * 4, "IDX")
        Wk3 = Wk.ap.rearrange("p (s k) -> p s k", k=4)
        IDX3 = IDX.ap.rearrange("p (s k) -> p s k", k=4)
        mark_all = A.off

        bones = A.f32(128, "bones")
        DMA(bones.ap, cd["bones"], [], [bones])
        bo64 = A.f32(128, "bo64")
        TS(bo64.ap, bones.ap, 1.0 / 64.0, None, ALU.mult, None, [bones], [bo64])
        mskA = A.f32(512, "mskA")
        DMA(mskA.ap, cd["mskA"], [], [mskA])
        mskB = A.f32(512, "mskB")
        DMA(mskB.ap, cd["mskB"], [], [mskB])
        striu = A.f32(128, "striu")
        DMA(striu.ap, cd["striu"], [], [striu])
        RmT = A.f32(128, "RmT")
        DMA(RmT.ap, cd["RmT"], [], [RmT])
        cmask = A.bf(2048, "cmask")
        ecap = A.f32(32, "ecap")
        DMA(ecap.ap, cd["ecap"], [], [ecap])
        vec = A.f32(42, "vec")
        DMA(vec.ap, vec_d, [], [vec])
        omm = A.f32(14, "omm")
        TS(omm.ap, vec.ap[:, 0:14], -1.0, 1.0, ALU.mult, ALU.add, [vec], [omm])
        V_W0, V_A0, V_KK, V_KA, V_RK, V_GG, V_GB = 14, 18, 22, 26, 30, 34, 38
        dup = A.f32(512, "dup")
        DMA(dup.ap, dup_d, [], [dup])
        gup = A.f32(512, "gup")
        DMA(gup.ap, gup_d, [], [gup])
        rw = A.f32(8 * 32, "rw")
        rw3 = rw.ap.rearrange("p (k e) -> p k e", k=8)
        DMA(rw3, rw_d.rearrange("(k p) e -> p k e", p=128), [], [rw])
        rb = A.f32(32, "rb")
        DMA(rb.ap, rb_d, [], [rb])
        Wbr = A.bf(4 * D, "Wbr")
        Wbr3 = Wbr.ap.rearrange("p (k c) -> p k c", k=4)
        DMA(Wbr3, wbr_d.rearrange("(k p) c -> p k c", p=128), [], [Wbr], eng="pool")
        Wba = A.bf(4 * D, "Wba")
        Wba3 = Wba.ap.rearrange("p (k c) -> p k c", k=4)
        DMA(Wba3, wba_d.rearrange("(k p) c -> p k c", p=128), [], [Wba], eng="pool")
        Wo = A.bf(8 * D, "Wo")
        Wo3 = Wo.ap.rearrange("p (k c) -> p k c", k=8)
        DMA(Wo3, wo_d.rearrange("(k p) c -> p k c", p=128), [], [Wo], eng="pool")
        km = [A.f32(16, f"km{p}") for p in range(4)]
        for p_ in range(4):
            MSET(km[p_].ap, 0.0, [km[p_]])
        Hs = [A.f32(128, f"H{p}") for p in range(4)]
        for p_ in range(4):
            MSET(Hs[p_].ap, 0.0, [Hs[p_]])
        carry = A.f32(14, "carry")
        MSET(carry.ap, 0.0, [carry])
        cnt = A.f32(32, "cnt")
        MSET(cnt.ap, 0.0, [cnt])
        jidx = A.f32(128, "jidx")
        P.op("pool", lambda e: e.iota(jidx.ap.rearrange("p (h j) -> p h j", h=8), pattern=[[0, 8], [1, 16]], base=0,
                                      channel_multiplier=0, allow_small_or_imprecise_dtypes=True), [], [jidx.b])
        xT = A.bf(8 * 512, "xT")
        xT3 = xT.ap.rearrange("p (k t) -> p k t", k=8)
        wring = [A.bf(8 * 128, f"wr{i}") for i in range(6)]
        wri = [0]
        xs_t = [A.f32(D, f"xs{i}") for i in range(2)]
        ropeC = A.f32(512, "ropeC")
        ropeS = A.f32(512, "ropeS")
        NSCR = 24
        scr = [A.f32(514, f"scr{i}") for i in range(NSCR)]
        free = list(range(NSCR))

        def b16(t):
            return t.ap.bitcast(BF16)[:, 0:512]

        def G():
            assert free, "scratch pool exhausted"
            return scr[free.pop(0)]

        def F(*ts):
            for t in ts:
                free.append(scr.index(t))

        yrT = A.bf(4 * 512, "yrT")
        yrT3 = yrT.ap.rearrange("p (k t) -> p k t", k=4)
        aoT = A.bf(4 * 512, "aoT")
        aoT3 = aoT.ap.rearrange("p (k t) -> p k t", k=4)
        mg = A.bf(8 * 512, "mg")
        mg3 = mg.ap.rearrange("p (k t) -> p k t", k=8)
        reg0 = A.off
        KTp = A.bf(T, "KTp")
        Vp = A.bf(NS * 2 * 66, "Vp")
        Vp4 = Vp.ap.rearrange("p (s h d) -> p s h d", s=NS, h=2)
        kst = A.bf(512, "kst")
        vst = A.bf(4 * 8 * 66, "vst")
        vst4 = vst.ap.rearrange("p (s h d) -> p s h d", s=4, h=8)
        QT = A.bf(4 * 512, "QT")
        QT3 = QT.ap.rearrange("p (k t) -> p k t", k=4)
        selm = A.f32(4 * 128, "selm")
        selm4 = selm.ap.rearrange("p (q h j) -> p q h j", q=4, h=8)
        acc = A.f32(2 * 4 * 66, "acc")
        acc4 = acc.ap.rearrange("p (h q d) -> p h q d", h=2, q=4)
        PTb = [A.bf(512, f"PT{i}") for i in range(8)]
        reg_att = A.off
        A.off = reg0
        CH = []
        for c4 in range(4):
            CH.append(dict(tok=A.bf(512, f"c_tok{c4}"), M2=A.bf(512, f"c_M2{c4}"), M3=A.bf(256, f"c_M3{c4}"),
                           Wa=A.bf(256, f"c_Wa{c4}"), Wb=A.bf(256, f"c_Wb{c4}"), NL=A.bf(512, f"c_NL{c4}"),
                           NL2=A.bf(512, f"c_NL2{c4}"), AbT=A.bf(128, f"c_AbT{c4}")))
        A.off = max(A.off, reg_att)
        x1b = A.bf(D, "x1b")
        for q4 in range(4):
            tq = G()
            DMA(tq.ap[:, 0:512], cd["cmask"][:, q4 * 512:(q4 + 1) * 512], [], [tq])
            CP(cmask.ap[:, q4 * 512:(q4 + 1) * 512], tq.ap[:, 0:512], [tq], [cmask])
            F(tq)
        sm = A.f32(256, "sm")
        print("stage A arena words", A.off)
        MSET(x1b.ap, 0.0, [x1b])
        zsrc = x1b.ap.rearrange("p (o c) -> p o c", o=1).to_broadcast([128, NBLK, D])
        for e_ in range(32):
            DMA(XE_d[e_ * CAP:(e_ + 1) * CAP, :].rearrange("(b p) c -> p b c", p=128), zsrc, [x1b], [BXE], eng="act")
        DMA(XE_d[NROW:NROW + 1, :], x1b.ap[0:1, :], [x1b], [BXE], eng="act")
        MSET(xs_t[0].ap, 0.0, [xs_t[0]])
        DMA(Y_d[NROW:NROW + 1, :], xs_t[0].ap[0:1, :], [xs_t[0]], [BY])

        for c in range(42):
            t = wring[c % len(wring)]
            v = t.ap.rearrange("p (k c) -> p k c", k=8)
            DMA(v, w_in_d.rearrange("(k p) c -> p k c", p=128)[:, :, c * 128:(c + 1) * 128], [], [t], eng="pool")
            DMA(Wb_d[c], t.ap, [t], [BWB[c]])

        def wchunk(c):
            t = wring[wri[0] % len(wring)]
            wri[0] += 1
            v = t.ap.rearrange("p (k c) -> p k c", k=8)
            DMA(t.ap, Wb_d[c], [BWB[c]], [t])
            return t, v

        def proj(c, ps):
            t, v = wchunk(c)
            for kc in range(8):
                MM(ps.ap, v[:, kc, :], xT3[:, kc, :], [t, xT], [ps], start=(kc == 0), stop=(kc == 7))

        def proj_shift(c):
            ps = PS()
            proj(c, ps)
            raw = G()
            CP(raw.ap[:, 0:1], carry.ap[:, c:c + 1], [carry], [raw], eng="pool")
            CP(raw.ap[:, 1:513], ps.ap, [ps], [raw], eng="act")
            CP(carry.ap[:, c:c + 1], raw.ap[:, 512:513], [raw], [carry], eng="pool")
            tmp = G()
            TS(tmp.ap[:, 0:512], raw.ap[:, 1:513], omm.ap[:, c:c + 1], None, ALU.mult, None, [raw, omm], [tmp])
            o = G()
            STT(o.ap[:, 0:512], raw.ap[:, 0:512], vec.ap[:, c:c + 1], tmp.ap[:, 0:512], ALU.mult, ALU.add, [raw, vec, tmp], [o])
            F(raw, tmp)
            return o

        def proj_shift_multi(chunks):
            pss = []
            for c in chunks:
                ps = PS()
                proj(c, ps)
                pss.append(ps)
            raws = []
            for c, ps in zip(chunks, pss):
                raw = G()
                CP(raw.ap[:, 0:1], carry.ap[:, c:c + 1], [carry], [raw], eng="pool")
                CP(raw.ap[:, 1:513], ps.ap, [ps], [raw], eng="act")
                raws.append(raw)
            for c, raw in zip(chunks, raws):
                CP(carry.ap[:, c:c + 1], raw.ap[:, 512:513], [raw], [carry], eng="pool")
            tmps = []
            for c, raw in zip(chunks, raws):
                tmp = G()
                TS(tmp.ap[:, 0:512], raw.ap[:, 1:513], omm.ap[:, c:c + 1], None, ALU.mult, None, [raw, omm], [tmp])
                tmps.append(tmp)
            outs = []
            for c, raw, tmp in zip(chunks, raws, tmps):
                o = G()
                STT(o.ap[:, 0:512], raw.ap[:, 0:512], vec.ap[:, c:c + 1], tmp.ap[:, 0:512], ALU.mult, ALU.add, [raw, vec, tmp], [o])
                outs.append(o)
            F(*raws, *tmps)
            return outs

        def ln_route(it, t0):
            P.phase = f"ln_route_t{it}"
            for qs in range(4):
                s = 4 * it + qs
                tsl = slice(t0 + qs * 128, t0 + (qs + 1) * 128)
                xs = xs_t[qs % 2]
                DMA(xs.ap, x_d[tsl, :], [], [xs])
                hpre = [G(), G()]
                x1t = [G(), G()]
                x1T = [G(), G()]
                for half in range(2):
                    ps = PS()
                    for kc in range(8):
                        MM(ps.ap, mg3[:, kc, qs * 128:(qs + 1) * 128], Wo3[:, kc, half * 512:(half + 1) * 512], [mg, Wo], [ps],
                           start=(kc == 0), stop=(kc == 7))
                    STT(hpre[half].ap[:, 0:512], xs.ap[:, half * 512:(half + 1) * 512], ALPHA, ps.ap, ALU.mult, ALU.add, [xs, ps], [hpre[half]])
                    yield
                layer_norm(P, TS, TT, ACT, RCP, hpre, x1t, lnp[0], lnp[1], sm)
                yield
                for half in range(2):
                    DMA(X1_d[tsl, half * 512:(half + 1) * 512], x1t[half].ap[:, 0:512], [x1t[half]], [BX1])
                    CP(x1b.ap[:, half * 512:(half + 1) * 512], x1t[half].ap[:, 0:512], [x1t[half]], [x1b], eng="act")
                for g4 in range(2):
                    ps = PS()
                    for j in range(4):
                        TR(ps.ap[:, j * 128:(j + 1) * 128], x1t[g4].ap[:, j * 128:(j + 1) * 128], identF.ap, [x1t[g4], identF], [ps])
                    CP(x1T[g4].ap[:, 0:512], ps.ap, [ps], [x1T[g4]], eng=("act" if g4 else "dve"))
                    yield
                CK(9, x1t[0].ap[:, 0:512], [x1t[0]])
                psl = PS()
                for kc in range(8):
                    MM(psl.ap[:, 0:32], x1T[kc // 4].ap[:, (kc % 4) * 128:(kc % 4 + 1) * 128], rw3[:, kc, :], [x1T[kc // 4], rw], [psl],
                       start=(kc == 0), stop=(kc == 7))
                F(*hpre, *x1T)
                lg = sm.ap[:, 16:48]
                TT(lg, psl.ap[:, 0:32], rb.ap, ALU.add, [psl, rb], [sm])
                yield
                m8 = sm.ap[:, 48:56]
                P.op("dve", lambda e, o=m8, i_=lg: e.max(out=o, in_=i_), [sm.b], [sm.b])
                nv0 = sm.ap[:, 56:57]
                TS(nv0, m8[:, 0:1], -1.0, None, ALU.mult, None, [sm], [sm])
                ev = sm.ap[:, 58:62]
                esum = sm.ap[:, 62:63]
                ACT(ev, m8[:, 0:4], AF.Exp, [sm], [sm], bias=nv0, accum=esum)
                RCP(esum, esum, [sm], [sm])
                TS(Wk3[:, s, :], ev, esum, None, ALU.mult, None, [sm], [Wk])
                yield
                mask = sm.ap[:, 64:96]
                TS(mask, lg, m8[:, 3:4], None, ALU.is_ge, None, [sm], [sm])
                psp = PS()
                MM(psp.ap[:, 0:32], striu.ap, mask, [striu, sm], [psp])
                MM(psp.ap[:, 32:64], onesF.ap, mask, [onesF, sm], [psp])
                slot = sm.ap[:, 96:128]
                ovf = sm.ap[:, 164:196]
                TT(slot, psp.ap[:, 0:32], cnt.ap, ALU.add, [psp, cnt], [sm])
                TS(ovf, slot, float(CAP), None, ALU.is_ge, None, [sm], [sm])
                TT(slot, slot, ecap.ap, ALU.add, [sm, ecap], [sm])
                TS(junk2 := sm.ap[:, 196:228], ovf, -1.0, 1.0, ALU.mult, ALU.add, [sm], [sm])
                TT(slot, slot, junk2, ALU.mult, [sm], [sm])
                STT(slot, ovf, float(NROW), slot, ALU.mult, ALU.add, [sm], [sm])
                TT(cnt.ap, cnt.ap, psp.ap[:, 32:64], ALU.add, [cnt, psp], [cnt])
                yield
                idxf = sm.ap[:, 128:132]
                junk = sm.ap[:, 132:164]
                for k in range(4):
                    STT(junk, lg, m8[:, k:k + 1], slot, ALU.is_equal, ALU.mult, [sm], [sm], accum=idxf[:, k:k + 1])
                CP(IDX3[:, s, :], idxf, [sm], [IDX])
                CK(10, sm.ap[:, 0:164], [sm])
                yield
                for k in range(4):
                    P.dma(lambda e, o=IDX3[:, s, k:k + 1]: e.indirect_dma_start(
                        out=XE_d, out_offset=bass.IndirectOffsetOnAxis(ap=o, axis=0), in_=x1b.ap, in_offset=None,
                        bounds_check=BC(e), oob_is_err=False), [x1b.b, IDX.b], [BXE], eng="pool")
                F(*x1t)
                yield
                CK(11, sm.ap[:, 0:164], [sm, BXE])
                CK(100 + it * 10 + qs, sm.ap[:, 0:164], [sm, BXE])

        pending = [None]

        def pump(n=1):
            g_ = pending[0]
            if g_ is None:
                return
            ph = P.phase
            for _ in range(n):
                try:
                    next(g_)
                except StopIteration:
                    pending[0] = None
                    break
            P.phase = ph

        CK(1, vec.ap, [vec])
        for it in range(NT):
            t0 = it * 512
            P.phase = f"a1_t{it}"
            for s in range(4):
                xs = xs_t[s % 2]
                DMA(xs.ap, x_d[t0 + s * 128:t0 + (s + 1) * 128, :], [], [xs])
                for g4 in range(2):
                    ps = PS()
                    for j in range(4):
                        kc = g4 * 4 + j
                        TR(ps.ap[:, j * 128:(j + 1) * 128], xs.ap[:, kc * 128:(kc + 1) * 128], identF.ap, [xs, identF], [ps])
                    CP(xT3[:, g4 * 4:(g4 + 1) * 4, s * 128:(s + 1) * 128], ps.ap.rearrange("p (k t) -> p k t", k=4), [ps], [xT],
                       eng=("act" if g4 else "dve"))
            DMA(ropeC.ap, cd["ropeC"][:, t0:t0 + 512], [], [ropeC])
            DMA(ropeS.ap, cd["ropeS"][:, t0:t0 + 512], [], [ropeS])

            CK(2, xT.ap.bitcast(F32)[:, 0:512], [xT])
            P.phase = f"rwkv_t{it}"
            sh12, sh13 = proj_shift_multi([12, 13])
            CK(3, sh12.ap[:, 0:512], [sh12])
            th = G()
            ACT(th.ap[0:64, 0:512], sh12.ap[0:64, 0:512], AF.Tanh, [sh12], [th])
            sg = G()
            ACT(sg.ap[:, 0:512], sh13.ap[:, 0:512], AF.Sigmoid, [sh13], [sg])
            F(sh13)
            for pr in range(4):
                pcs = slice(pr * 128, (pr + 1) * 128)
                ps_d, ps_a, ps_g = PS(), PS(), PS()
                MM(ps_d.ap, dup.ap[0:64, pcs], th.ap[0:64, 0:512], [dup, th], [ps_d])
                MM(ps_a.ap, dup.ap[64:128, pcs], sh12.ap[64:128, 0:512], [dup, sh12], [ps_a])
                MM(ps_g.ap, gup.ap[:, pcs], sg.ap[:, 0:512], [gup, sg], [ps_g])
                sgw, aT, gT = G(), G(), G()
                ACT(sgw.ap[:, 0:512], ps_d.ap, AF.Sigmoid, [ps_d, vec], [sgw], bias=vec.ap[:, V_W0 + pr:V_W0 + pr + 1])
                ACT(aT.ap[:, 0:512], ps_a.ap, AF.Sigmoid, [ps_a, vec], [aT], bias=vec.ap[:, V_A0 + pr:V_A0 + pr + 1])
                CP(gT.ap[:, 0:512], ps_g.ap, [ps_g], [gT], eng="act")
                Ls = G()
                for c4 in range(4):
                    cs = slice(c4 * 128, (c4 + 1) * 128)
                    P.op("dve", lambda e, o=Ls.ap[:, cs], d1=sgw.ap[:, cs]: e.tensor_tensor_scan(o, onesF.ap, d1, 0.0, ALU.mult, ALU.add),
                         [onesF.b, sgw.b], [Ls.b])
                Lp = G()
                TT(Lp.ap[:, 0:512], Ls.ap[:, 0:512], sgw.ap[:, 0:512], ALU.subtract, [Ls, sgw], [Lp])
                gC, E1, E2, E3 = G(), G(), G(), G()
                ACT(gC.ap[:, 0:4], Ls.ap[:, 0:512].rearrange("p (c t) -> p c t", c=4)[:, :, 127], AF.Exp, [Ls], [gC], scale=-C0)
                ACT(E1.ap[:, 0:512], Ls.ap[:, 0:512], AF.Exp, [Ls], [E1], scale=-C0)
                ACT(E2.ap[:, 0:512], Ls.ap[:, 0:512], AF.Exp, [Ls], [E2], scale=C0)
                ACT(E3.ap[:, 0:512], Lp.ap[:, 0:512], AF.Exp, [Lp], [E3], scale=-C0)
                F(sgw, Ls, Lp)
                pump()
                rS, kS, vS = proj_shift_multi([pr, 4 + pr, 8 + pr])
                pump()
                kk, t1 = G(), G()
                TS(kk.ap[:, 0:512], kS.ap[:, 0:512], vec.ap[:, V_KK + pr:V_KK + pr + 1], None, ALU.mult, None, [kS, vec], [kk])
                TS(t1.ap[:, 0:512], aT.ap[:, 0:512], 1.0, vec.ap[:, V_KA + pr:V_KA + pr + 1], ALU.subtract, ALU.mult, [aT, vec], [t1])
                Rt = G()
                TT(b16(Rt), rS.ap[:, 0:512], E1.ap[:, 0:512], ALU.mult, [rS, E1], [Rt])
                sq = G()
                TT(sq.ap[:, 0:512], kk.ap[:, 0:512], kk.ap[:, 0:512], ALU.mult, [kk], [sq])
                kmod = G()
                STT(kmod.ap[:, 0:512], t1.ap[:, 0:512], 1.0, kS.ap[:, 0:512], ALU.add, ALU.mult, [t1, kS], [kmod])
                F(kS, E1)
                ps_n = PS()
                MM(ps_n.ap, bones.ap, sq.ap[:, 0:512], [bones, sq], [ps_n])
                STT(t1.ap[:, 0:512], rS.ap[:, 0:512], vec.ap[:, V_RK + pr:V_RK + pr + 1], kmod.ap[:, 0:512], ALU.mult, ALU.mult, [rS, vec, kmod], [t1])
                Kt = G()
                TT(b16(Kt), kmod.ap[:, 0:512], E2.ap[:, 0:512], ALU.mult, [kmod, E2], [Kt])
                TS(sq.ap[:, 0:512], ps_n.ap, 1e-24, None, ALU.max, None, [ps_n], [sq])
                ACT(sq.ap[:, 0:512], sq.ap[:, 0:512], AF.Ln, [sq], [sq])
                ps_b = PS()
                MM(ps_b.ap, bones.ap, t1.ap[:, 0:512], [bones, t1], [ps_b])
                F(rS, kmod)
                pump()
                ACT(sq.ap[:, 0:512], sq.ap[:, 0:512], AF.Exp, [sq], [sq], scale=-0.5)
                bon = G()
                TT(bon.ap[:, 0:512], ps_b.ap, vS.ap[:, 0:512], ALU.mult, [ps_b, vS], [bon])
                TT(kk.ap[:, 0:512], kk.ap[:, 0:512], sq.ap[:, 0:512], ALU.mult, [kk, sq], [kk])
                F(sq, t1)
                At = G()
                STT(b16(At), kk.ap[:, 0:512], -1.0, E3.ap[:, 0:512], ALU.mult, ALU.mult, [kk, E3], [At])
                bT = G()
                TT(bT.ap[:, 0:512], kk.ap[:, 0:512], aT.ap[:, 0:512], ALU.mult, [kk, aT], [bT])
                F(E3, aT, kk)
                Bt = G()
                TT(b16(Bt), bT.ap[:, 0:512], E2.ap[:, 0:512], ALU.mult, [bT, E2], [Bt])
                F(bT, E2)
                vSb, Hb = G(), G()
                CP(b16(vSb), vS.ap[:, 0:512], [vS], [vSb], eng="pool")
                CP(Hb.ap.bitcast(BF16)[:, 0:128], Hs[pr].ap, [Hs[pr]], [Hb], eng="pool")
                CK(4, At.ap[:, 0:512], [At])
                pump()
                gmk = G()
                for c4 in range(4):
                    TS(gmk.ap[:, c4 * 128:(c4 + 1) * 128], bones.ap, gC.ap[:, c4:c4 + 1], None, ALU.mult, None, [bones, gC], [gmk], eng="pool")
                psY = psb[7]
                H = Hs[pr]
                m_su_sl = mskA.ap.rearrange("p (a b c) -> p a b c", a=2, b=2)[:, :, 0, :]
                m_su_sui = mskB.ap.rearrange("p (a b c) -> p a b c", a=2, b=2)[:, :, 0, :]
                CSL = [slice(c4 * 128, (c4 + 1) * 128) for c4 in range(4)]
                st = [dict(CH[c4]) for c4 in range(4)]
                for c4 in range(4):
                    cs, c = CSL[c4], st[c4]
                    ps = PS()
                    psv = ps.ap.bitcast(BF16)
                    for j, src in enumerate((Kt, Bt, At, vSb)):
                        TR(psv[:, j * 128:(j + 1) * 128], b16(src)[:, cs], identB.ap, [src, identB], [ps])
                    CP(c["tok"].ap, psv[:, 0:512], [ps], [c["tok"]], eng="act")
                    c["tk"] = c["tok"].ap.rearrange("p (j c) -> p j c", j=4)
                for c4 in range(4):
                    cs, c = CSL[c4], st[c4]
                    NL, M2, M3 = c["NL"], c["M2"], c["M3"]
                    NLv = NL.ap.rearrange("p (a h c) -> p a h c", a=2, h=2)
                    M2v = M2.ap.rearrange("p (a h c) -> p a h c", a=2, h=2)
                    for hh in range(2):
                        hp = slice(64 * hh, 64 * hh + 64)
                        pa, pb = PS(), PS()
                        MM(pa.ap[:, 0:128], b16(Bt)[hp, cs], b16(At)[hp, cs], [Bt, At], [pa])
                        MM(pa.ap[:, 128:256], b16(At)[hp, cs], b16(Bt)[hp, cs], [Bt, At], [pa])
                        MM(pa.ap[:, 256:384], b16(Kt)[hp, cs], b16(At)[hp, cs], [Kt, At], [pa])
                        MM(pa.ap[:, 384:512], b16(Bt)[hp, cs], b16(Rt)[hp, cs], [Bt, Rt], [pa])
                        MM(pb.ap[:, 0:128], b16(Kt)[hp, cs], b16(Rt)[hp, cs], [Kt, Rt], [pb])
                        TT(NLv[:, :, hh, :], pa.ap[:, 0:256].rearrange("p (a c) -> p a c", a=2), m_su_sl, ALU.mult, [pa, mskA], [NL])
                        TT(M2v[:, :, hh, :], pa.ap[:, 256:512].rearrange("p (a c) -> p a c", a=2), m_su_sui, ALU.mult, [pa, mskB], [M2],
                           eng=("dve" if hh else "pool") if False else "dve")
                        TT(M3.ap[:, hh * 128:hh * 128 + 128], pb.ap[:, 0:128], mskB.ap[:, 256:384], ALU.mult, [pb, mskB], [M3])
                    pump()
                for c4 in range(4):
                    c = st[c4]
                    tk, M2, Wa = c["tk"], c["M2"], c["Wa"]
                    ps4 = PS()
                    for hh in range(2):
                        MM(ps4.ap[:, 64 + hh * 64:128 + hh * 64], M2.ap[:, hh * 128:hh * 128 + 128], tk[:, 3, hh * 64:hh * 64 + 64], [M2, c["tok"]], [ps4])
                    CP(Wa.ap[:, 0:64], tk[:, 2, 0:64], [c["tok"]], [Wa], eng="pool")
                    CP(Wa.ap[:, 192:256], tk[:, 2, 64:128], [c["tok"]], [Wa], eng="pool")
                    CP(Wa.ap[:, 64:192], ps4.ap[:, 64:192], [ps4], [Wa], eng="act")
                for j in range(7):
                    pump()
                    for c4 in range(4):
                        c = st[c4]
                        NL, Wa, Wb = c["NL"], c["Wa"], c["Wb"]
                        ps5 = PS()
                        for hh in range(2):
                            MM(ps5.ap[:, hh * 128:hh * 128 + 128], NL.ap[:, hh * 128:hh * 128 + 128], Wa.ap[:, hh * 128:hh * 128 + 128], [NL, Wa], [ps5])
                        TT(Wb.ap, ps5.ap[:, 0:256], Wa.ap, ALU.add, [ps5, Wa], [Wb])
                        c["Wa"], c["Wb"] = Wb, Wa
                    if j < 6:
                        for c4 in range(4):
                            c = st[c4]
                            NL, NL2 = c["NL"], c["NL2"]
                            ps6 = PS()
                            for hh in range(2):
                                o0, o1 = hh * 128, 256 + hh * 128
                                MM(ps6.ap[:, o0:o0 + 128], NL.ap[:, o1:o1 + 128], NL.ap[:, o0:o0 + 128], [NL], [ps6])
                                MM(ps6.ap[:, o1:o1 + 128], NL.ap[:, o0:o0 + 128], NL.ap[:, o1:o1 + 128], [NL], [ps6])
                            CP(NL2.ap, ps6.ap, [ps6], [NL2], eng="act")
                            c["NL"], c["NL2"] = NL2, NL
                for c4 in range(4):
                    c = st[c4]
                    Wa, AbT = c["Wa"], c["AbT"]
                    ps7 = PS()
                    p7v = ps7.ap.bitcast(BF16)
                    for hh in range(2):
                        TR(p7v[:, hh * 128:hh * 128 + 128], Wa.ap[:, hh * 128:hh * 128 + 128], identB.ap, [Wa, identB], [ps7])
                    CP(AbT.ap[0:64, 0:128], p7v[0:64, 0:128], [ps7], [AbT], eng="act")
                    CP(AbT.ap[64:128, 0:128], p7v[64:128, 128:256], [ps7], [AbT], eng="act")
                for c4 in range(4):
                    cs, c = CSL[c4], st[c4]
                    tk, M2, M3, Wa, AbT = c["tk"], c["M2"], c["M3"], c["Wa"], c["AbT"]
                    psU = PS()
                    Hbv = Hb.ap.bitcast(BF16)[:, 0:128]
                    MM(psU.ap[:, 0:128], AbT.ap[:, 0:128], Hbv, [AbT, Hb], [psU])
                    Ut = G()
                    U = Tl(Ut.ap.bitcast(BF16), Ut.b)
                    TT(U.ap[:, 0:128], psU.ap[:, 0:128], Wa.ap[:, 64:192], ALU.add, [psU, Wa], [U])
                    MM(psY.ap[:, cs], Hbv, b16(Rt)[:, cs], [Hb, Rt], [psY], start=True, stop=False)
                    for hh in range(2):
                        hp = slice(64 * hh, 64 * hh + 64)
                        yo = psY.ap[hp, cs]
                        MM(yo, U.ap[:, hh * 64:hh * 64 + 64], M2.ap[:, 256 + hh * 128:256 + hh * 128 + 128], [U, M2], [psY],
                           start=False, stop=False, tp=(0, 64 * hh))
                        MM(yo, tk[:, 3, hh * 64:hh * 64 + 64], M3.ap[:, hh * 128:hh * 128 + 128], [c["tok"], M3], [psY],
                           start=False, stop=True, tp=(0, 64 * hh))
                    psH = PS()
                    MM(psH.ap[:, 0:128], tk[:, 0, :], tk[:, 3, :], [c["tok"]], [psH], start=True, stop=False)
                    MM(psH.ap[:, 0:128], tk[:, 1, :], U.ap[:, 0:128], [c["tok"], U], [psH], start=False, stop=True)
                    tmpH = G()
                    TT(tmpH.ap[:, 0:128], psH.ap[:, 0:128], H.ap, ALU.add, [psH, H], [tmpH])
                    TT(Hbv, tmpH.ap[:, 0:128], gmk.ap[:, c4 * 128:(c4 + 1) * 128], ALU.mult, [tmpH, gmk], [Hb])
                    TT(H.ap, tmpH.ap[:, 0:128], gmk.ap[:, c4 * 128:(c4 + 1) * 128], ALU.mult, [tmpH, gmk], [H], eng="pool")
                    F(tmpH, Ut)
                    pump()
                F(Rt, Kt, Bt, At, gC, vSb, Hb, gmk)
                Yt = G()
                CP(Yt.ap[:, 0:512], psY.ap, [psY], [Yt], eng="act")
                ps = PS()
                MM(ps.ap, bo64.ap, Yt.ap[:, 0:512], [bo64, Yt], [ps])
                TT(Yt.ap[:, 0:512], Yt.ap[:, 0:512], ps.ap, ALU.subtract, [Yt, ps], [Yt])
                sq2 = G()
                TT(sq2.ap[:, 0:512], Yt.ap[:, 0:512], Yt.ap[:, 0:512], ALU.mult, [Yt], [sq2], eng="pool")
                ps = PS()
                MM(ps.ap, bo64.ap, sq2.ap[:, 0:512], [bo64, sq2], [ps])
                TS(sq2.ap[:, 0:512], ps.ap, GN_EPS, None, ALU.add, None, [ps], [sq2])
                ACT(sq2.ap[:, 0:512], sq2.ap[:, 0:512], AF.Ln, [sq2], [sq2])
                ACT(sq2.ap[:, 0:512], sq2.ap[:, 0:512], AF.Exp, [sq2], [sq2], scale=-0.5)
                TT(Yt.ap[:, 0:512], Yt.ap[:, 0:512], sq2.ap[:, 0:512], ALU.mult, [Yt, sq2], [Yt])
                TS(Yt.ap[:, 0:512], Yt.ap[:, 0:512], vec.ap[:, V_GG + pr:V_GG + pr + 1], vec.ap[:, V_GB + pr:V_GB + pr + 1], ALU.mult, ALU.add, [Yt, vec], [Yt])
                TT(Yt.ap[:, 0:512], Yt.ap[:, 0:512], bon.ap[:, 0:512], ALU.add, [Yt, bon], [Yt])
                TT(yrT3[:, pr, :], Yt.ap[:, 0:512], gT.ap[:, 0:512], ALU.mult, [Yt, gT], [yrT])
                F(Yt, sq2, bon, gT, vS)
            F(sh12, th, sg)
            CK(6, yrT.ap.bitcast(F32)[:, 0:512], [yrT])
            CK(60 + it, yrT.ap.bitcast(F32)[:, 0:512], [yrT])

            pump(10 ** 6)
            P.phase = f"attnprep_t{it}"
            P.barrier()
            MSET(vst.ap, 1.0, [vst])
            qr = [None] * 4
            for g2 in range(2):
                prs = (2 * g2, 2 * g2 + 1)
                keys = [(pr, which) for pr in prs for which in range(3)]
                pss, fs, rps, t2s, vps = {}, {}, {}, {}, {}
                for key in keys:
                    pss[key] = PS()
                    proj(14 + 4 * key[1] + key[0], pss[key])
                for key in keys:
                    fs[key] = G()
                    CP(fs[key].ap[:, 0:512], pss[key].ap, [pss[key]], [fs[key]], eng="act")
                for pr in prs:
                    for which in (0, 1):
                        rps[(pr, which)] = PS()
                        MM(rps[(pr, which)].ap, RmT.ap, fs[(pr, which)].ap[:, 0:512], [RmT, fs[(pr, which)]], [rps[(pr, which)]])
                for pr in prs:
                    vps[pr] = PS()
                    fv = fs[(pr, 2)]
                    for s in range(4):
                        TR(vps[pr].ap[:, s * 128:(s + 1) * 128], fv.ap[:, s * 128:(s + 1) * 128], identF.ap, [fv, identF], [vps[pr]])
                for key, ps in rps.items():
                    f = fs[key]
                    t2s[key] = G()
                    TT(t2s[key].ap[:, 0:512], ps.ap, ropeS.ap, ALU.mult, [ps, ropeS], [t2s[key]])
                    TT(f.ap[:, 0:512], f.ap[:, 0:512], ropeC.ap, ALU.mult, [f, ropeC], [f], eng="pool")
                for pr in prs:
                    CP(vst4[:, :, 2 * pr:2 * pr + 2, 0:64], vps[pr].ap.rearrange("p (s h d) -> p s h d", s=4, h=2), [vps[pr]], [vst])
                    F(fs[(pr, 2)])
                for key in rps:
                    f = fs[key]
                    TT(f.ap[:, 0:512], f.ap[:, 0:512], t2s[key].ap[:, 0:512], ALU.add, [f, t2s[key]], [f])
                    F(t2s[key])
                for pr in prs:
                    fq, fk = fs[(pr, 0)], fs[(pr, 1)]
                    CP(QT3[:, pr, :], fq.ap[:, 0:512], [fq], [QT], eng="act")
                    qr[pr] = fq
                    P.op("dve", lambda e, o=km[pr].ap[:, 2 * it:2 * it + 2], i_=fk.ap[:, 0:512].rearrange("p (b t) -> p b t", b=2):
                         e.tensor_reduce(o, i_, AX.X, ALU.add), [fk.b], [km[pr].b])
                    TS(km[pr].ap[:, 2 * it:2 * it + 2], km[pr].ap[:, 2 * it:2 * it + 2], 1.0 / 256.0, None, ALU.mult, None, [km[pr]], [km[pr]])
                    CP(kst.ap, fk.ap[:, 0:512], [fk], [kst], eng="act")
                    DMA(KT_d[pr][:, t0:t0 + 512], kst.ap, [kst], [BKT])
                    F(fk)
            DMA(V_d[4 * it:4 * it + 4].rearrange("s p c -> p s c"), vst.ap.rearrange("p (s c) -> p s c", s=4), [vst], [BV])
            stat = {}
            for ob in (2 * it, 2 * it + 1):
                t_ = G()
                TS(t_.ap[:, 0:128], jidx.ap, float(ob), -1e30, ALU.is_ge, ALU.mult, [jidx], [t_])
                TS(t_.ap[:, 128:256], jidx.ap, float(ob), None, ALU.is_lt, None, [jidx], [t_])
                TS(t_.ap[:, 256:384], jidx.ap, float(ob), None, ALU.is_equal, None, [jidx], [t_])
                stat[ob] = t_
            gms = [G() for _ in range(4)]
            for grp in ((0, 1), (2, 3)):
                psgs = {}
                for qs in grp:
                    psgs[qs] = [PS(), PS()]
                    for hh in range(2):
                        hp = slice(64 * hh, 64 * hh + 64)
                        for pr in range(4):
                            MM(psgs[qs][hh].ap[:, pr * 16:pr * 16 + 16], qr[pr].ap[hp, qs * 128:(qs + 1) * 128], km[pr].ap[hp, 0:16],
                               [qr[pr], km[pr]], [psgs[qs][hh]])
                for qs in grp:
                    ob = 2 * it + qs // 2
                    gmv = gms[qs].ap[:, 0:128].rearrange("p (r h j) -> p r h j", r=4, h=2)
                    ngv = stat[ob].ap[:, 0:128].rearrange("p (r h j) -> p r h j", r=4, h=2)
                    for hh in range(2):
                        TT(gmv[:, :, hh, :], ngv[:, :, hh, :], psgs[qs][hh].ap[:, 0:64].rearrange("p (r j) -> p r j", r=4), ALU.add,
                           [stat[ob], psgs[qs][hh]], [gms[qs]])
            for h in range(8):
                for qs in range(4):
                    gm3 = gms[qs].ap[:, 0:128].rearrange("p (h j) -> p h j", h=8)
                    m8 = gms[qs].ap[:, 128:192].rearrange("p (h j) -> p h j", h=8)
                    P.op("dve", lambda e, o=m8[:, h, :], i_=gm3[:, h, :]: e.max(out=o, in_=i_), [gms[qs].b], [gms[qs].b])
            for qs in range(4):
                gm3 = gms[qs].ap[:, 0:128].rearrange("p (h j) -> p h j", h=8)
                m8 = gms[qs].ap[:, 128:192].rearrange("p (h j) -> p h j", h=8)
                sel = gms[qs].ap[:, 192:320].rearrange("p (h j) -> p h j", h=8)
                TT(sel, gm3, m8[:, :, 2:3].to_broadcast([128, 8, 16]), ALU.is_ge, [gms[qs]], [gms[qs]])
            for qs in range(4):
                ob = 2 * it + qs // 2
                TT(gms[qs].ap[:, 192:320], gms[qs].ap[:, 192:320], stat[ob].ap[:, 128:256], ALU.mult, [gms[qs], stat[ob]], [gms[qs]])
            for qs in range(4):
                ob = 2 * it + qs // 2
                sel = gms[qs].ap[:, 192:320].rearrange("p (h j) -> p h j", h=8)
                TT(selm4[:, qs, :, :], sel, stat[ob].ap[:, 256:384].rearrange("p (h j) -> p h j", h=8), ALU.add, [gms[qs], stat[ob]], [selm])
            F(*gms, *stat.values())
            for f in qr:
                F(f)
            P.phase = f"attn_t{it}"
            nk = (it + 1) * 512
            pti_ = [0]

            def att_s1(pr, j):
                pts = {}
                pss = {}
                for u in range(2):
                    kt = 2 * j + u
                    for hh in range(2):
                        hp = slice(64 * hh, 64 * hh + 64)
                        pss[(hh, u)] = PS()
                        MM(pss[(hh, u)].ap, KTp.ap[hp, kt * 128:(kt + 1) * 128], QT3[hp, pr, :], [KTp, QT], [pss[(hh, u)]])
                for u in range(2):
                    kt = 2 * j + u
                    kl = kt - 4 * it
                    for hh in range(2):
                        pt = PTb[pti_[0] % 8]
                        pti_[0] += 1
                        ACT(pt.ap, pss[(hh, u)].ap, AF.Exp, [pss[(hh, u)]], [pt], scale=0.125)
                        if kl >= 0:
                            TT(pt.ap, pt.ap, cmask.ap[:, kl * 512:(kl + 1) * 512], ALU.mult, [pt, cmask], [pt])
                        pts[(hh, u)] = pt
                return pts

            def att_s2(pr, j, pts):
                q0 = 2 if j == 2 * it + 1 else 0
                nq = 4 - q0
                for hh in range(2):
                    h = 2 * pr + hh
                    psO = PS()
                    for qs in range(q0, 4):
                        for u in range(2):
                            kt = 2 * j + u
                            MM(psO.ap[:, qs * 128:qs * 128 + 65], pts[(hh, u)].ap[:, qs * 128:(qs + 1) * 128], Vp4[:, kt, hh, 0:65],
                               [pts[(hh, u)], Vp], [psO], start=(u == 0), stop=(u == 1))
                    tmp = G()
                    tv = tmp.ap[:, 0:nq * 65].rearrange("p (q d) -> p q d", q=nq)
                    TT(tv, psO.ap.rearrange("p (q d) -> p q d", q=4)[:, q0:4, 0:65],
                       selm4[:, q0:4, h, j:j + 1].to_broadcast([128, nq, 65]), ALU.mult, [psO, selm], [tmp])
                    TT(acc4[:, hh, q0:4, 0:65], acc4[:, hh, q0:4, 0:65], tv, ALU.add, [acc, tmp], [acc])
                    F(tmp)

            for pr in range(4):
                DMA(KTp.ap[:, 0:nk], KT_d[pr][:, 0:nk], [BKT], [KTp])
                DMA(Vp4[:, 0:4 * (it + 1), :, :],
                    V_d[0:4 * (it + 1)].rearrange("s p (h d) -> p s h d", h=8)[:, :, 2 * pr:2 * pr + 2, :], [BV], [Vp])
                MSET(acc.ap, 0.0, [acc])
                nj = 2 * it + 2
                nxt = att_s1(pr, 0)
                for j in range(nj):
                    cur = nxt
                    if j + 1 < nj:
                        nxt = att_s1(pr, j + 1)
                    att_s2(pr, j, cur)
                rden = G()
                rd3 = rden.ap[:, 0:8].rearrange("p (h q) -> p h q", h=2)
                RCP(rd3, acc4[:, :, :, 64], [acc], [rden])
                atq = G()
                atv = atq.ap[:, 0:512].rearrange("p (q h d) -> p q h d", q=4, h=2)
                for qs in range(4):
                    TT(atv[:, qs, :, :], acc4[:, :, qs, 0:64], rd3[:, :, qs:qs + 1].to_broadcast([128, 2, 64]), ALU.mult, [acc, rden], [atq])
                ps = PS()
                for qs in range(4):
                    TR(ps.ap[:, qs * 128:(qs + 1) * 128], atq.ap[:, qs * 128:(qs + 1) * 128], identF.ap, [atq, identF], [ps])
                CP(aoT3[:, pr, :], ps.ap, [ps], [aoT], eng="act")
                F(rden, atq)

            CK(7, aoT.ap.bitcast(F32)[:, 0:512], [aoT])
            CK(70 + it, aoT.ap.bitcast(F32)[:, 0:512], [aoT])
            P.barrier()
            P.phase = f"merge_t{it}"
            for cc in range(8):
                ccs = slice(cc * 128, (cc + 1) * 128)
                psr, psa = PS(), PS()
                for pr in range(4):
                    MM(psr.ap, Wbr3[:, pr, ccs], yrT3[:, pr, :], [Wbr, yrT], [psr], start=(pr == 0), stop=(pr == 3))
                for pr in range(4):
                    MM(psa.ap, Wba3[:, pr, ccs], aoT3[:, pr, :], [Wba, aoT], [psa], start=(pr == 0), stop=(pr == 3))
                ps = PS()
                proj(26 + cc, ps)
                g0 = G()
                ACT(g0.ap[:, 0:512], ps.ap, AF.Sigmoid, [ps], [g0])
                ps = PS()
                proj(34 + cc, ps)
                g1 = G()
                ACT(g1.ap[:, 0:512], ps.ap, AF.Sigmoid, [ps], [g1])
                TT(g0.ap[:, 0:512], g0.ap[:, 0:512], psr.ap, ALU.mult, [g0, psr], [g0])
                TT(g1.ap[:, 0:512], g1.ap[:, 0:512], psa.ap, ALU.mult, [g1, psa], [g1])
                TT(mg3[:, cc, :], g0.ap[:, 0:512], g1.ap[:, 0:512], ALU.add, [g0, g1], [mg], eng="pool")
                F(g0, g1)
            CK(8, mg.ap.bitcast(F32)[:, 0:512], [mg])
            pending[0] = ln_route(it, t0)
            if it == NT - 1 or stop != 0:
                pump(10 ** 6)

        if stage == 1:
            P.barrier()
            for s in range(NS):
                t = xs_t[s % 2]
                DMA(t.ap, X1_d[s * 128:(s + 1) * 128, :], [BX1], [t])
                DMA(out_d[s * 128:(s + 1) * 128, :], t.ap, [t], [Bout])
            P.barrier()
            P.emit()
            return nc

        P.barrier()
        A.off = mark_all
        WinB = [A.bf(8 * 2048, f"Win{i}") for i in range(2)]
        WoutB = [A.bf(8 * D, f"Wout{i}") for i in range(2)]
        binB = A.f32(32 * 16, "bin")
        DMA(binB.ap, ebin_d, [], [binB])
        bin3 = binB.ap.rearrange("p (e f) -> p e f", e=32)
        boutB = [A.f32(D, f"bout{i}") for i in range(2)]
        XK = [A.bf(NBLK * D, f"xe_tok{i}") for i in range(2)]
        xeT = A.bf(8 * CAP, "xeT")
        xeT3 = xeT.ap.rearrange("p (k s) -> p k s", k=8)
        actT = A.bf(8 * CAP, "actT")
        actT3 = actT.ap.rearrange("p (k s) -> p k s", k=8)
        eg = [A.f32(HALF, f"eg{i}") for i in range(2)]
        es = [A.f32(HALF, f"es{i}") for i in range(2)]
        el = [A.f32(HALF, f"el{i}") for i in range(2)]
        yo = [A.f32(D, f"yo{i}") for i in range(2)]
        print("stage B arena words", A.off)
        psbB = [Tl(t.ap.bitcast(BF16), t.b) for t in psb]
        SR = [A.f32(2048, f"stg{i}") for i in range(3)]
        sri = [0]

        def make_loader(e_):
            Win, Wout, bout = WinB[e_ % 2], WoutB[e_ % 2], boutB[e_ % 2]
            Win3 = Win.ap.rearrange("p (k f) -> p k f", k=8)
            Wout3 = Wout.ap.rearrange("p (k c) -> p k c", k=8)
            pieces = []
            for kc in range(8):
                pieces.append((Win3[:, kc, :], ewin_d[e_][kc * 128:(kc + 1) * 128, :], Win, None))
            wo_src = ewout_d[e_].rearrange("(k p) c -> p k c", p=128)
            for j in range(4):
                pieces.append((Wout3[:, 2 * j:2 * j + 2, :], wo_src[:, 2 * j:2 * j + 2, :], Wout, 2))
            stg = []

            def issue(i):
                dst, src, tl, k2 = pieces[i]
                t = SR[sri[0] % 3]
                sri[0] += 1
                v = t.ap if k2 is None else t.ap.rearrange("p (k c) -> p k c", k=2)
                DMA(v, src, [], [t], eng="pool")
                stg.append((t, v))

            def start():
                DMA(bout.ap, ebout_d[e_:e_ + 1, :].partition_broadcast(128), [], [bout])
                issue(0)
                issue(1)

            def step(i):
                if i >= len(pieces):
                    return
                dst, src, tl, k2 = pieces[i]
                t, v = stg[i]
                CP(dst, v, [t], [tl], eng="act")
                if i + 2 < len(pieces):
                    issue(i + 2)

            return start, step

        def xe_load(e_):
            xk = XK[e_ % 2]
            DMA(xk.ap.rearrange("p (b c) -> p b c", b=NBLK), XE_d[e_ * CAP:(e_ + 1) * CAP, :].rearrange("(b p) c -> p b c", p=128), [BXE], [xk])

        def xe_transpose(e_):
            xk = XK[e_ % 2]
            xk3 = xk.ap.rearrange("p (b c) -> p b c", b=NBLK)
            for b_ in range(NBLK):
                for g4 in range(2):
                    ps = psb[psi[0] % 8]
                    psB = psbB[psi[0] % 8]
                    psi[0] += 1
                    for j in range(4):
                        kc = g4 * 4 + j
                        TR(psB.ap[:, j * 128:(j + 1) * 128], xk3[:, b_, kc * 128:(kc + 1) * 128], identB.ap, [xk, identB], [ps])
                    CP(xeT3[:, g4 * 4:(g4 + 1) * 4, b_ * 128:(b_ + 1) * 128], psB.ap[:, 0:512].rearrange("p (k t) -> p k t", k=4), [ps], [xeT],
                       eng=("act" if g4 else "dve"))

        xe_load(0)
        xe_transpose(0)
        P.phase = "expert0"
        ld_start, ld_step = make_loader(0)
        ld_start()
        for i in range(12):
            ld_step(i)
        for e_ in range(32):
            P.phase = f"expert{e_}"
            if e_ + 1 < 32:
                ld_start, ld_step = make_loader(e_ + 1)
                ld_start()
            else:
                ld_step = lambda i: None
            unit = [0]
            Win, Wout, bout = WinB[e_ % 2], WoutB[e_ % 2], boutB[e_ % 2]
            Win3 = Win.ap.rearrange("p (k f) -> p k f", k=8)
            Wout3 = Wout.ap.rearrange("p (k c) -> p k c", k=8)
            if e_ + 1 < 32:
                xe_load(e_ + 1)
            for fc in range(8):
                for hf in range(2):
                    ss = slice(hf * HALF, (hf + 1) * HALF)
                    pg, pl = PS(), PS()
                    for kc in range(8):
                        MM(pg.ap[:, 0:HALF], Win3[:, kc, fc * 128:(fc + 1) * 128], xeT3[:, kc, ss], [Win, xeT], [pg], start=(kc == 0), stop=(kc == 7))
                    for kc in range(8):
                        MM(pl.ap[:, 0:HALF], Win3[:, kc, 1024 + fc * 128:1024 + (fc + 1) * 128], xeT3[:, kc, ss], [Win, xeT], [pl], start=(kc == 0), stop=(kc == 7))
                    g_, s_, l_ = eg[hf], es[hf], el[hf]
                    TS(g_.ap, pg.ap[:, 0:HALF], bin3[:, e_, fc:fc + 1], 7.0, ALU.add, ALU.min, [pg, binB], [g_])
                    ACT(s_.ap, g_.ap, AF.Sigmoid, [g_], [s_], scale=1.702)
                    TS(l_.ap, pl.ap[:, 0:HALF], bin3[:, e_, 8 + fc:9 + fc], 7.0, ALU.add, ALU.min, [pl, binB], [l_])
                    TS(l_.ap, l_.ap, -7.0, 1.0, ALU.max, ALU.add, [l_], [l_])
                    TT(g_.ap, g_.ap, s_.ap, ALU.mult, [g_, s_], [g_])
                    TT(actT3[:, fc, ss], g_.ap, l_.ap, ALU.mult, [g_, l_], [actT])
                    ld_step(unit[0])
                    unit[0] += 1
            if e_ + 1 < 32:
                xe_transpose(e_ + 1)
            for b_ in range(NBLK):
                y_ = yo[b_ % 2]
                for half in range(2):
                    ps = PS()
                    hs = slice(half * 512, (half + 1) * 512)
                    for fc in range(8):
                        MM(ps.ap, actT3[:, fc, b_ * 128:(b_ + 1) * 128], Wout3[:, fc, hs], [actT, Wout], [ps], start=(fc == 0), stop=(fc == 7))
                    TT(y_.ap[:, hs], ps.ap, bout.ap[:, hs], ALU.add, [ps, bout], [y_])
                DMA(Y_d[e_ * CAP + b_ * 128:e_ * CAP + (b_ + 1) * 128, :], y_.ap, [y_], [BY])

        P.barrier()
        P.phase = "combine"
        A.off = mark_all
        yk = [A.f32(D, f"yk{i}") for i in range(8)]
        x1c = [A.f32(D, f"x1c{i}") for i in range(2)]
        oc = [A.f32(D, f"oc{i}") for i in range(2)]
        sm2 = A.f32(64, "sm2")
        lnp2 = [A.f32(D, f"lnq{i}") for i in range(2)]
        for i in range(2):
            DMA(lnp2[i].ap, lnp_d[2 + i], [], [lnp2[i]])
        print("stage C arena words", A.off)

        def c_load(s):
            tsl = slice(s * 128, (s + 1) * 128)
            DMA(x1c[s % 2].ap, X1_d[tsl, :], [BX1], [x1c[s % 2]])
            for k in range(4):
                y_ = yk[(s % 2) * 4 + k]

                def gath(e, o=y_.ap, ix=IDX3[:, s, k:k + 1]):
                    return e.indirect_dma_start(out=o, out_offset=None, in_=Y_d, in_offset=bass.IndirectOffsetOnAxis(ap=ix, axis=0),
                                                bounds_check=BC(e), oob_is_err=False)
                P.dma(gath, [BY, IDX.b], [y_.b], eng="pool")

        c_load(0)
        for s in range(NS):
            tsl = slice(s * 128, (s + 1) * 128)
            if s + 1 < NS:
                c_load(s + 1)
            xc, o_ = x1c[s % 2], oc[s % 2]
            TS(xc.ap, xc.ap, ALPHA, None, ALU.mult, None, [xc], [xc])
            for k in range(4):
                y_ = yk[(s % 2) * 4 + k]
                STT(xc.ap, y_.ap, Wk3[:, s, k:k + 1], xc.ap, ALU.mult, ALU.add, [y_, Wk, xc], [xc])
            xh = [Tl(xc.ap[:, 0:512], xc.b), Tl(xc.ap[:, 512:1024], xc.b)]
            oh = [Tl(o_.ap[:, 0:512], o_.b), Tl(o_.ap[:, 512:1024], o_.b)]
            layer_norm(P, TS, TT, ACT, RCP, xh, oh, lnp2[0], lnp2[1], sm2)
            DMA(out_d[tsl, :], o_.ap, [o_], [Bout])
        P.barrier()
        P.emit()
    return nc


def layer_norm(P, TS, TT, ACT, RCP, src, dst, g, b, sm):
    st6 = sm.ap[:, 0:12].rearrange("p (c s) -> p c s", c=2)
    for c in range(2):
        P.op("dve", lambda e, o=st6[:, c, :], i_=src[c].ap[:, 0:512]: e.bn_stats(o, i_), [src[c].b], [sm.b])
    mv = sm.ap[:, 12:14]
    P.op("dve", lambda e: e.bn_aggr(mv, sm.ap[:, 0:12]), [sm.b], [sm.b])
    sd = sm.ap[:, 14:15]
    TS(sd, mv[:, 1:2], LN_EPS, None, ALU.add, None, [sm], [sm])
    ACT(sd, sd, AF.Ln, [sm], [sm])
    ACT(sd, sd, AF.Exp, [sm], [sm], scale=-0.5)
    for c in range(2):
        cs = slice(c * 512, (c + 1) * 512)
        TS(dst[c].ap[:, 0:512], src[c].ap[:, 0:512], mv[:, 0:1], sd, ALU.subtract, ALU.mult, [src[c], sm], [dst[c]])
        TT(dst[c].ap[:, 0:512], dst[c].ap[:, 0:512], g.ap[:, cs], ALU.mult, [dst[c], g], [dst[c]], eng="pool")
        TT(dst[c].ap[:, 0:512], dst[c].ap[:, 0:512], b.ap[:, cs], ALU.add, [dst[c], b], [dst[c]])


def prep_weights(inp):
    f = lambda a: np.ascontiguousarray(np.asarray(a, dtype=np.float32))
    w = {}
    w["w_in"] = f(inp["w_in"][0])
    cols = [f(inp["shift_mix"][0]).reshape(14, 128).T]
    for k in ("decay_w0", "iclr_a0", "k_k", "k_a", "r_k", "gn_g", "gn_b"):
        cols.append(f(inp[k][0]).reshape(4, 128).T)
    w["vec"] = f(np.concatenate(cols, 1))
    w["dup"] = f(np.concatenate([inp["decay_up"][0], inp["iclr_up"][0]], 0))
    w["gate_up"] = f(inp["gate_up"][0])
    w["w_branch_rwkv"] = f(inp["w_branch_rwkv"][0])
    w["w_branch_attn"] = f(inp["w_branch_attn"][0])
    w["w_out"] = f(inp["w_out"][0])
    lnp = np.stack([inp["ln1_g"][0], inp["ln1_b"][0], inp["ln2_g"][0], inp["ln2_b"][0]], 0)
    w["lnp"] = f(np.broadcast_to(np.asarray(lnp)[:, None, :], (4, 128, D)))
    w["router_w"] = f(inp["router_w"][0])
    w["router_b_bc"] = f(np.broadcast_to(np.asarray(inp["router_b"][0])[None, :], (128, 32)))
    w["expert_w_in"] = f(inp["expert_w_in"][0])
    bi = f(inp["expert_b_in"][0]).reshape(32, 16, 128)
    w["expert_b_in_fm"] = f(bi.transpose(2, 0, 1).reshape(128, 32 * 16))
    w["expert_w_out"] = f(inp["expert_w_out"][0])
    w["expert_b_out"] = f(inp["expert_b_out"][0])
    return w


_NC_CACHE = {}


def kernel(**inputs):
    x = np.asarray(inputs["x"], dtype=np.float32)
    B, T, _ = x.shape
    w = prep_weights(inputs)
    for k, v in host_consts(T).items():
        w["c_" + k] = np.ascontiguousarray(v, dtype=np.float32)
    if T not in _NC_CACHE:
        _NC_CACHE[T] = build(T)
    nc = _NC_CACHE[T]
    in_maps = []
    for b in range(B):
        m = dict(w)
        m["x"] = np.ascontiguousarray(x[b])
        in_maps.append(m)
    res = run_bass_kernel_spmd(nc, in_maps, core_ids=list(range(B)))
    return np.stack([np.asarray(r["out"], dtype=np.float32) for r in res.results], 0)
```
